# Optimizing a Trainium2 kernel written in Bass

```python
import jax, jax.numpy as jnp
from jax import lax
import numpy as np

D_MODEL = 2048
BATCH = 2
SEQ = 16384
DEPTH = 1

GRID_W = 64
D_MIX = 2048
DN_HEADS = 8
DN_DK = 128
DN_DV = 128
DN_QK_WIDTH = DN_HEADS * DN_DK
DN_V_WIDTH = DN_HEADS * DN_DV
CONV_K = 5
CHUNK = 64
ATT_HEADS = 8
ATT_KV_HEADS = 2
ATT_HD = 128
ATT_WIDTH = ATT_HEADS * ATT_HD
ATT_KV_WIDTH = ATT_KV_HEADS * ATT_HD
ROPE_THETA = 10000.0
Q_BLOCK = 128
N_EXPERTS = 16
EXPERT_FF = 2048
CAPACITY_FACTOR = 2
EPS = 1e-6

_IN_SIZES = (DN_QK_WIDTH, DN_QK_WIDTH, DN_V_WIDTH, DN_V_WIDTH, 2 * DN_HEADS, 2 * DN_HEADS,
             ATT_WIDTH, ATT_KV_WIDTH, ATT_KV_WIDTH)
IN_COLS = sum(_IN_SIZES)
_IN_SPLITS = tuple(int(v) for v in np.cumsum(_IN_SIZES)[:-1])
CONV_CH = 2 * DN_QK_WIDTH + DN_V_WIDTH

kernel_name = "hybrid_deltanet_gqa_axial_ec_moe_encoder"


def rmsnorm(x, w):
    xf = x.astype(jnp.float32)
    y = xf * lax.rsqrt(jnp.mean(xf * xf, axis=-1, keepdims=True) + EPS)
    return (y * w.astype(jnp.float32)).astype(x.dtype)


def l2norm(x):
    xf = x.astype(jnp.float32)
    return xf * lax.rsqrt(jnp.sum(xf * xf, axis=-1, keepdims=True) + EPS)


def centred_short_conv(x, w):
    pad = (CONV_K - 1) // 2
    y = lax.conv_general_dilated(
        x, w[:, None, :].astype(x.dtype), window_strides=(1,), padding=[(pad, pad)],
        dimension_numbers=("NWC", "WIO", "NWC"), feature_group_count=x.shape[-1])
    return jax.nn.silu(y)


def gated_delta_chunked(q, k, v, log_alpha, beta):
    B, S, H, dk = q.shape
    dv = v.shape[-1]
    n = S // CHUNK

    def to_chunks(t):
        t = t.reshape((B, n, CHUNK, H) + t.shape[3:])
        return jnp.moveaxis(t, 3, 1)

    q, k, v = to_chunks(q), to_chunks(k), to_chunks(v)
    beta = to_chunks(beta)
    g = jnp.cumsum(to_chunks(log_alpha), axis=-1)
    pos = jnp.arange(CHUNK)
    lower_incl = pos[:, None] >= pos[None, :]
    strict = pos[:, None] > pos[None, :]
    decay = jnp.exp(jnp.where(lower_incl, g[..., :, None] - g[..., None, :], -jnp.inf))

    k_beta = k * beta[..., None]
    L = jnp.where(strict, jnp.einsum("bhncd,bhnmd->bhncm", k_beta, k) * decay, 0.0)
    t_sys = jnp.eye(CHUNK, dtype=q.dtype) + L
    u = lax.linalg.triangular_solve(t_sys, v * beta[..., None], left_side=True, lower=True,
                                    unit_diagonal=True)
    w = lax.linalg.triangular_solve(t_sys, k_beta * jnp.exp(g)[..., None], left_side=True,
                                    lower=True, unit_diagonal=True)
    qk = jnp.einsum("bhncd,bhnmd->bhncm", q, k) * decay
    q_dec = q * jnp.exp(g)[..., None]
    g_last = g[..., -1]
    k_dec = k * jnp.exp(g_last[..., None] - g)[..., None]

    def step(state, xs):
        u_c, w_c, qk_c, qd_c, kd_c, gl_c = xs
        v_new = u_c - jnp.einsum("bhcd,bhde->bhce", w_c, state)
        o = (jnp.einsum("bhcd,bhde->bhce", qd_c, state)
             + jnp.einsum("bhcm,bhme->bhce", qk_c, v_new))
        state = state * jnp.exp(gl_c)[..., None, None] + jnp.einsum("bhcd,bhce->bhde", kd_c, v_new)
        return state, o

    xs = tuple(jnp.moveaxis(t, 2, 0) for t in (u, w, qk, q_dec, k_dec, g_last))
    s0 = jnp.zeros((B, H, dk, dv), jnp.float32)
    _, o = lax.scan(step, s0, xs)
    o = jnp.moveaxis(o, 0, 2)
    return jnp.moveaxis(o, 1, 3).reshape(B, S, H, dv)


def deltanet_group(q, k, v, z, a, b, conv_w, a_log, dt_bias, o_norm_w):
    B, S, _ = q.shape
    qkv = centred_short_conv(jnp.concatenate([q, k, v], axis=-1), conv_w)
    q = qkv[..., :DN_QK_WIDTH]
    k = qkv[..., DN_QK_WIDTH:2 * DN_QK_WIDTH]
    v = qkv[..., 2 * DN_QK_WIDTH:]
    q = l2norm(q.reshape(B, S, DN_HEADS, DN_DK)) * (DN_DK ** -0.5)
    k = l2norm(k.reshape(B, S, DN_HEADS, DN_DK))
    v = v.reshape(B, S, DN_HEADS, DN_DV).astype(jnp.float32)
    a = a.astype(jnp.float32).reshape(B, S, 2, DN_HEADS)
    b = b.astype(jnp.float32).reshape(B, S, 2, DN_HEADS)
    log_alpha = -jnp.exp(a_log.astype(jnp.float32)) * jax.nn.softplus(a + dt_bias.astype(jnp.float32))
    beta = jax.nn.sigmoid(b)
    o_fwd = gated_delta_chunked(q, k, v, log_alpha[:, :, 0], beta[:, :, 0])
    flip = lambda t: jnp.flip(t, axis=1)
    o_bwd = flip(gated_delta_chunked(flip(q), flip(k), flip(v),
                                     flip(log_alpha[:, :, 1]), flip(beta[:, :, 1])))
    o = rmsnorm(o_fwd + o_bwd, o_norm_w) * jax.nn.silu(
        z.reshape(B, S, DN_HEADS, DN_DV).astype(jnp.float32))
    return o.reshape(B, S, DN_V_WIDTH).astype(z.dtype)


def axial_rope_tables(seq):
    rows = seq // GRID_W
    row = jnp.repeat(jnp.arange(rows, dtype=jnp.float32), GRID_W)
    col = jnp.tile(jnp.arange(GRID_W, dtype=jnp.float32), rows)
    n_freq = ATT_HD // 4
    inv_freq = ROPE_THETA ** (-jnp.arange(n_freq, dtype=jnp.float32) / n_freq)
    ang_r = row[:, None] * inv_freq
    ang_c = col[:, None] * inv_freq
    ang = jnp.concatenate([ang_r, ang_r, ang_c, ang_c], axis=-1)
    return jnp.cos(ang), jnp.sin(ang)


def apply_axial_rope(x, cos, sin):
    x1, x2, x3, x4 = jnp.split(x, 4, axis=-1)
    rot = jnp.concatenate([-x2, x1, -x4, x3], axis=-1)
    return (x.astype(jnp.float32) * cos + rot.astype(jnp.float32) * sin).astype(x.dtype)


def gqa_axial_group(q, k, v, q_norm_w, k_norm_w):
    B, S, _ = q.shape
    G = ATT_HEADS // ATT_KV_HEADS
    q = q.reshape(B, S, ATT_KV_HEADS, G, ATT_HD)
    k = k.reshape(B, S, ATT_KV_HEADS, ATT_HD)
    v = v.reshape(B, S, ATT_KV_HEADS, ATT_HD)
    cos, sin = axial_rope_tables(S)
    q = apply_axial_rope(rmsnorm(q, q_norm_w), cos[:, None, None], sin[:, None, None])
    k = apply_axial_rope(rmsnorm(k, k_norm_w), cos[:, None], sin[:, None])
    scale = ATT_HD ** -0.5
    nb = S // Q_BLOCK
    qb = jnp.moveaxis(q.reshape(B, nb, Q_BLOCK, ATT_KV_HEADS, G, ATT_HD), 1, 0)

    def block(q_blk):
        s = jnp.einsum("bqkgd,bskd->bkgqs", q_blk, k).astype(jnp.float32) * scale
        p = jax.nn.softmax(s, axis=-1)
        return jnp.einsum("bkgqs,bskd->bqkgd", p.astype(v.dtype), v)

    o = lax.map(block, qb)
    return jnp.moveaxis(o, 0, 1).reshape(B, S, ATT_WIDTH)


def expert_choice_moe(h, w_router, w_gate, w_up, w_down):
    B, S, D = h.shape
    cap = CAPACITY_FACTOR * S // N_EXPERTS
    logits = jnp.einsum("bsd,de->bse", h, w_router).astype(jnp.float32)
    aff = jax.nn.softmax(logits, axis=-1)
    gates, idx = lax.top_k(jnp.swapaxes(aff, 1, 2), cap)
    xs = jax.vmap(lambda hb, ib: hb[ib])(h, idx)
    a = jnp.einsum("becd,edf->becf", xs, w_gate)
    u = jnp.einsum("becd,edf->becf", xs, w_up)
    y = jnp.einsum("becf,efd->becd", jax.nn.silu(a) * u, w_down) * gates[..., None].astype(h.dtype)
    return jax.vmap(lambda ib, yb: jnp.zeros((S, D), yb.dtype).at[ib.reshape(-1)].add(
        yb.reshape(-1, D)))(idx, y)


def setup_inputs(seed: int = 0) -> dict:
    key = jax.random.key(seed)
    ks = jax.random.split(key, 18)
    f32 = jnp.float32
    nrm = lambda k, shape, scale: jax.random.normal(k, shape, f32) * scale
    x = jax.random.normal(ks[0], (BATCH, SEQ, D_MODEL), f32)
    ln1_w = 1.0 + nrm(ks[1], (DEPTH, D_MODEL), 0.02)
    w_in = nrm(ks[2], (DEPTH, D_MODEL, IN_COLS), D_MODEL ** -0.5)
    conv_w = nrm(ks[3], (DEPTH, CONV_K, CONV_CH), CONV_K ** -0.5)
    a_log = jnp.log(jax.random.uniform(ks[4], (DEPTH, 2, DN_HEADS), f32, 1.0, 16.0))
    dt = jnp.exp(jax.random.uniform(ks[5], (DEPTH, 2, DN_HEADS), f32, jnp.log(1e-3), jnp.log(1e-1)))
    dt_bias = dt + jnp.log(-jnp.expm1(-dt))
    dn_norm_w = 1.0 + nrm(ks[6], (DEPTH, DN_DV), 0.02)
    q_norm_w = 1.0 + nrm(ks[7], (DEPTH, ATT_HD), 0.02)
    k_norm_w = 1.0 + nrm(ks[8], (DEPTH, ATT_HD), 0.02)
    w_out = nrm(ks[9], (DEPTH, D_MIX, D_MODEL), D_MIX ** -0.5)
    ln2_w = 1.0 + nrm(ks[10], (DEPTH, D_MODEL), 0.02)
    w_router = nrm(ks[11], (DEPTH, D_MODEL, N_EXPERTS), D_MODEL ** -0.5)
    w_gate = nrm(ks[12], (DEPTH, N_EXPERTS, D_MODEL, EXPERT_FF), D_MODEL ** -0.5)
    w_up = nrm(ks[13], (DEPTH, N_EXPERTS, D_MODEL, EXPERT_FF), D_MODEL ** -0.5)
    w_down = nrm(ks[14], (DEPTH, N_EXPERTS, EXPERT_FF, D_MODEL), EXPERT_FF ** -0.5)
    return {"x": x, "ln1_w": ln1_w, "w_in": w_in, "conv_w": conv_w, "a_log": a_log,
            "dt_bias": dt_bias, "dn_norm_w": dn_norm_w, "q_norm_w": q_norm_w,
            "k_norm_w": k_norm_w, "w_out": w_out, "ln2_w": ln2_w, "w_router": w_router,
            "w_gate": w_gate, "w_up": w_up, "w_down": w_down}


def reference(x, ln1_w, w_in, conv_w, a_log, dt_bias, dn_norm_w, q_norm_w, k_norm_w,
              w_out, ln2_w, w_router, w_gate, w_up, w_down):
    for l in range(DEPTH):
        h = rmsnorm(x, ln1_w[l])
        proj = jnp.einsum("bsd,dc->bsc", h, w_in[l])
        dq, dk, dv, dz, da, db, aq, ak, av = jnp.split(proj, _IN_SPLITS, axis=-1)
        o_a = deltanet_group(dq, dk, dv, dz, da, db, conv_w[l], a_log[l], dt_bias[l], dn_norm_w[l])
        o_b = gqa_axial_group(aq, ak, av, q_norm_w[l], k_norm_w[l])
        mixed = jnp.concatenate([o_a, o_b], axis=-1)
        x = x + jnp.einsum("bsc,cd->bsd", mixed, w_out[l])
        h = rmsnorm(x, ln2_w[l])
        x = x + expert_choice_moe(h, w_router[l], w_gate[l], w_up[l], w_down[l])
    return x
```

```python
import math
import numpy as np
import concourse.bass as bass
import concourse.mybir as mybir
from concourse.bass_utils import run_bass_kernel_spmd

F32 = mybir.dt.float32
BF16 = mybir.dt.bfloat16
I32 = mybir.dt.int32
U8 = mybir.dt.uint8
ALU = mybir.AluOpType
AF = mybir.ActivationFunctionType
AX = mybir.AxisListType
DSIZE = {F32: 4, BF16: 2, I32: 4, U8: 1}

D = 2048
EPS = 1e-6
NE = 16
BIG = float(1 << 20)


class Buf:
    __slots__ = ("lw", "rd")

    def __init__(self):
        self.lw = None
        self.rd = []


class Tile:
    __slots__ = ("ap", "bufs", "excl")

    def __init__(self, ap, bufs, excl=False):
        self.ap = ap
        self.bufs = bufs
        self.excl = excl

    def __getitem__(self, k):
        return self.ap[k]


class DT:
    def __init__(self, ap):
        self.ap = ap
        self._b = {}

    def b(self, key=0):
        if key not in self._b:
            self._b[key] = Tile(None, [Buf()])
        return self._b[key]

    def all(self):
        return list(self._b.values())

    def __getitem__(self, k):
        return self.ap[k]


class Arena:
    def __init__(self, nc, name, nbytes, gran=512):
        self.ap = nc.alloc_sbuf_tensor(name, [128, nbytes], U8).ap()
        self.gran = gran
        self.nbytes = nbytes
        self.bufs = [Buf() for _ in range((nbytes + gran - 1) // gran)]
        self.off = 0

    def alloc(self, shape, dt):
        n = 1
        for s in shape[1:]:
            n *= s
        nb = n * DSIZE[dt]
        al = 512 if nb >= 512 else 32
        off = (self.off + al - 1) // al * al
        assert off + nb <= self.nbytes, f"SBUF arena overflow {off}+{nb}"
        self.off = off + nb
        v = self.ap[0:shape[0], off:off + nb].bitcast(dt)
        if len(shape) == 3:
            v = v.rearrange("p (a b) -> p a b", a=shape[1])
        elif len(shape) == 4:
            v = v.rearrange("p (a b c) -> p a b c", a=shape[1], b=shape[2])
        g0, g1 = off // self.gran, (off + nb - 1) // self.gran
        return Tile(v, self.bufs[g0:g1 + 1])

    def mark(self):
        return self.off

    def release(self, m):
        self.off = m


class PsumArena:
    def __init__(self, nc):
        self.banks = [nc.alloc_psum_tensor(f"pbank{i}", [128, 512], F32).ap() for i in range(8)]
        self.bufs = [[Buf()] for _ in range(8)]

    def tile(self, bank, off_bytes, shape, dt):
        n = 1
        for s in shape[1:]:
            n *= s
        nb = n * DSIZE[dt]
        assert off_bytes % 4 == 0 and off_bytes + nb <= 2048
        v = self.banks[bank][0:shape[0], off_bytes // 4:(off_bytes + nb + 3) // 4]
        if dt != F32:
            v = v.bitcast(dt)
        if len(shape) == 3:
            v = v.rearrange("p (a b) -> p a b", a=shape[1])
        return Tile(v, self.bufs[bank], excl=True)


class Sched:
    NDMA = 16
    scopes = False
    LIM = 3000

    def __init__(self, nc):
        self.nc = nc
        self.ops = []
        self.phase = "init"
        self.phases = []

    @staticmethod
    def _flat(ts):
        out = []
        for t in ts:
            if isinstance(t, Buf):
                out.append(t)
            else:
                out.extend(t.bufs)
        return out

    def op(self, eng, fn, R=(), W=(), dma=False):
        R = list(R)
        W = list(W) + [t for t in R if isinstance(t, Tile) and t.excl]
        self.ops.append((eng, fn, self._flat(R), self._flat(W), dma))
        self.phases.append(self.phase)

    def dma(self, q, out, in_, R=(), W=(), **kw):
        self.op(q, lambda e: e.dma_start(out=out, in_=in_, **kw), R, W, dma=True)

    def cc(self, fn, R=(), W=()):
        self.op("pq", fn, R, W, dma="cc")

    def emit(self):
        nc = self.nc
        ops = self.ops
        n = len(ops)
        stream_of = {"pe": "pe", "act": "act", "dve": "dve", "pool": "pool",
                     "sp": "sp", "pq": "pool", "aq": "act"}
        handles = {"pe": nc.tensor, "act": nc.scalar, "dve": nc.vector, "pool": nc.gpsimd,
                   "sp": nc.sync}
        deps = [None] * n
        signal = [False] * n
        for i, (eng, fn, reads, writes, dma) in enumerate(ops):
            d = set()
            raw = set()
            for b in reads:
                if b.lw is not None:
                    d.add(b.lw)
                    raw.add(b.lw)
            for b in writes:
                if b.lw is not None:
                    d.add(b.lw)
                d.update(b.rd)
            for b in writes:
                b.lw = i
                b.rd = []
            if not dma:
                st_i = stream_of[eng]
                for b in reads:
                    if b.lw != i:
                        b.rd = [j for j in b.rd if ops[j][4] or stream_of[ops[j][0]] != st_i]
                        b.rd.append(i)
            else:
                for b in reads:
                    if b.lw != i:
                        b.rd.append(i)
            d.discard(i)
            st = stream_of[eng]
            dd = []
            for j in d:
                ej, _, _, _, dj = ops[j]
                if dj or stream_of[ej] != st or (j in raw and st != "pe"):
                    dd.append(j)
                    signal[j] = True
            deps[i] = dd
            if dma:
                signal[i] = True
        streams = ("pe", "act", "dve", "pool", "sp")
        sems = {s: nc.alloc_semaphore(name=f"s_{s}_0") for s in streams}
        epoch = {s: 0 for s in streams}
        used_q = {eng for (eng, _, _, _, dma) in ops if dma and dma != "cc"}
        dsems = {q: [nc.alloc_semaphore(name=f"d_{q}{k}") for k in range(self.NDMA)]
                 for q in ("sp", "pq", "aq") if q in used_q}
        cnt = {s: 0 for s in streams}
        dcnt = {q: 0 for q in dsems}
        semval = [None] * n
        ccsem = nc.alloc_semaphore(name="ccsem")
        ccn = 0
        for i, (eng, fn, reads, writes, dma) in enumerate(ops):
            if dma == "cc":
                ccn += 1
                semval[i] = (ccsem, ccn, ("cc", 0, 0))
            elif dma:
                k = dcnt[eng] % self.NDMA
                semval[i] = (dsems[eng][k], 16 * (dcnt[eng] // self.NDMA + 1), ("d", eng, k))
                dcnt[eng] += 1
            elif signal[i]:
                st = stream_of[eng]
                if cnt[st] >= self.LIM:
                    epoch[st] += 1
                    sems[st] = nc.alloc_semaphore(name=f"s_{st}_{epoch[st]}")
                    cnt[st] = 0
                cnt[st] += 1
                semval[i] = (sems[st], cnt[st], ("c", st, epoch[st]))
        seen = {s: {} for s in streams}
        dma_hist = {q: [] for q in dsems}
        for i, (eng, fn, reads, writes, dma) in enumerate(ops):
            st = stream_of[eng]
            h = handles[st]
            waits = {}
            for j in deps[i]:
                sem, v, key = semval[j]
                if waits.get(key, (None, 0))[1] < v:
                    waits[key] = (sem, v)
            if dma and dma != "cc":
                hist = dma_hist[eng]
                if len(hist) >= self.NDMA:
                    sem, v, key = semval[hist[-self.NDMA]]
                    if waits.get(key, (None, 0))[1] < v:
                        waits[key] = (sem, v)
                hist.append(i)
            for key, (sem, v) in waits.items():
                if seen[st].get(key, 0) < v:
                    h.wait_ge(sem, v)
                    seen[st][key] = v
            if self.scopes and (i == 0 or self.phases[i] != self.phases[i - 1]):
                if i > 0:
                    nc.pop_named_scope(self.phases[i - 1])
                nc.push_named_scope(self.phases[i])
            inst = fn(h)
            if semval[i] is not None:
                sem, v, key = semval[i]
                if dma == "cc":
                    inst.then_inc(sem)
                else:
                    inst.then_inc(sem, 16 if dma else 1)
        if self.scopes and n:
            nc.pop_named_scope(self.phases[n - 1])
        last = {}
        for i in range(n):
            if semval[i] is not None:
                sem, v, key = semval[i]
                if last.get(key, (None, 0))[1] < v:
                    last[key] = (sem, v)
        for key, (sem, v) in last.items():
            if seen["sp"].get(key, 0) < v:
                nc.sync.wait_ge(sem, v)
        self.stats = dict(n_ops=n, cnt=cnt, dcnt=dcnt)


class _Stop(Exception):
    pass


def build(S, stop=None, dump=()):
    NT = S // 128
    NB = S // 512
    TPC = S // 4
    NTC = TPC // 128
    CAP = 2 * S // NE
    TBK = min(512, CAP)
    GRP = [[0, 1, 2, 3], [4, 5, 6, 7]]
    TQ = min(1024, TPC)
    NQ = S // TQ
    HQ = 256

    nc = bass.Bass("TRN2", target_bir_lowering=False)
    SC = Sched(nc)
    op, dma = SC.op, SC.dma

    def din(name, shape, dt=F32):
        return DT(nc.dram_tensor(name, shape, dt, kind="ExternalInput").ap())

    scr = {}

    def dscr(name, shape, dt=F32):
        t = DT(nc.dram_tensor(name, shape, dt).ap())
        scr[name] = t
        return t

    xT = din("xT", [D, S])
    xtok = din("xtok", [TPC, D])
    w_in = din("w_in", [D, 1544])
    ln1 = din("ln1", [128, 16])
    convw = din("convw", [128, 6, 5])
    alog = din("alog", [128, 4])
    dtb = din("dtb", [128, 4])
    dnw = din("dnw", [128, 128])
    qnw = din("qnw", [128, 1])
    knw = din("knw", [128, 1])
    qnrow = din("qnrow", [128, 128])
    knrow = din("knrow", [128, 128])
    cosT = din("cosT", [128, S])
    sinT = din("sinT", [128, S])
    w_out = din("w_out", [D, D])
    ln2bc = din("ln2bc", [128, D])
    wr = din("wr", [128, 16, NE])
    wg = din("wg", [4, D, D])
    wu = din("wu", [4, D, D])
    wd = din("wd", [4, D, D])
    cmask = din("cmask", [128, 4, 128])
    cmisc = din("cmisc", [128, 4, 128])
    cci = din("cci", [128, 2])
    cecap = din("cecap", [128, NE])
    out = DT(nc.dram_tensor("out", [TPC, D], F32, kind="ExternalOutput").ap())

    cm_raw = dscr("cm_raw", [9, 128, S + 4])
    tm_raw = dscr("tm_raw", [S, 392])
    dn_qT = dscr("dn_qT", [2, 128, S], BF16)
    dn_kT = dscr("dn_kT", [2, 128, S], BF16)
    dn_ktm = dscr("dn_ktm", [2, S, 128], BF16)
    dn_vtm = dscr("dn_vtm", [2, S, 128], BF16)
    dn_o = dscr("dn_o", [4, S, 128])
    mixT_loc = dscr("mixT_loc", [NQ, 512, TQ], BF16)
    mixT_full = dscr("mixT_full", [NQ, 2048, TQ], BF16)
    x1_loc = dscr("x1_loc", [TPC, D])
    h2_loc = dscr("h2_loc", [TPC, D], BF16)
    h2_full = dscr("h2_full", [S, D], BF16)
    aff_loc = dscr("aff_loc", [TPC, NE])
    aff_full = dscr("aff_full", [S, NE])
    pos_d = dscr("pos_d", [128, NE, NT], I32)
    posg_d = dscr("posg_d", [128, NE, NT], I32)
    gs_d = dscr("gs_d", [128, NE, NT])
    xs = [dscr(f"xs{j}", [CAP, D], BF16) for j in range(4)]
    y_loc = dscr("y_loc", [4 * CAP, D], BF16)
    y_full = dscr("y_full", [NE * CAP, D], BF16)

    AR = Arena(nc, "arena", 200 * 1024)
    PS = PsumArena(nc)

    masks = AR.alloc([128, 4, 128], F32)
    misc = AR.alloc([128, 4, 128], F32)
    miscb = AR.alloc([128, 4, 128], BF16)
    ci = AR.alloc([128, 2], F32)
    epst = AR.alloc([128, 1], F32)
    la_all = AR.alloc([128, NT, 4], F32)
    be_all = AR.alloc([128, NT, 4], F32)
    nbe_all = AR.alloc([128, NT, 4], F32)
    dma("sp", masks.ap, cmask.ap, W=[masks])
    dma("sp", misc.ap, cmisc.ap, W=[misc])
    dma("sp", ci.ap, cci.ap, W=[ci])
    op("pool", lambda e: e.tensor_copy(out=miscb.ap, in_=misc.ap), [misc], [miscb])
    op("pool", lambda e: e.memset(epst.ap, EPS), [], [epst])
    LI, SL, UI, SU = (masks.ap[:, i, :] for i in range(4))
    identF, rotF, UFf, onesF = (misc.ap[:, i, :] for i in range(4))
    identB, UFb, onesB = miscb.ap[:, 0, :], miscb.ap[:, 2, :], miscb.ap[:, 3, :]
    BASE = AR.mark()

    _regs = {}

    def bcreg(e, val):
        if val not in _regs:
            _regs[val] = e.to_reg(val)
        return _regs[val]

    _dyn = {}

    def dynval(e, mul, add):
        ek = id(e)
        if ("r", ek) not in _dyn:
            _dyn[("r", ek)] = e.snap(e.partition_id() % 4)
        key = (ek, mul, add)
        if key not in _dyn:
            _dyn[key] = e.snap(_dyn[("r", ek)] * mul + add)
        return _dyn[key]

    def interleave(gens):
        lists = []
        for g in gens:
            so, sp_ = SC.ops, SC.phases
            SC.ops, SC.phases = [], []
            g()
            lists.append((SC.ops, SC.phases))
            SC.ops, SC.phases = so, sp_
        idx = [0] * len(lists)
        more = True
        while more:
            more = False
            for k, (lo, lp) in enumerate(lists):
                if idx[k] < len(lo):
                    SC.ops.append(lo[idx[k]])
                    SC.phases.append(lp[idx[k]])
                    idx[k] += 1
                    more = True

    def mm(o, lhsT, rhs, R, W, start=True, stop=True):
        op("pe", lambda e: e.matmul(o, lhsT=lhsT, rhs=rhs, start=start, stop=stop), R, W)

    def tr(o, in_, ident, R, W):
        op("pe", lambda e: e.transpose(out=o, in_=in_, identity=ident), R, W)

    def act(o, in_, func, R, W, bias=None, scale=None, accum=None):
        kw = {}
        if bias is not None:
            kw["bias"] = bias
        if scale is not None:
            kw["scale"] = scale
        if accum is not None:
            kw["accum_out"] = accum
        op("act", lambda e: e.activation(out=o, in_=in_, func=func, **kw), R, W)

    def tt(eng, o, a, b, alu, R, W):
        op(eng, lambda e: e.tensor_tensor(out=o, in0=a, in1=b, op=alu), R, W)

    def ts(eng, o, a, s1, alu1, R, W, s2=None, alu2=None):
        if alu2 is None:
            op(eng, lambda e: e.tensor_scalar(out=o, in0=a, scalar1=s1, scalar2=None, op0=alu1), R, W)
        else:
            op(eng, lambda e: e.tensor_scalar(out=o, in0=a, scalar1=s1, scalar2=s2, op0=alu1, op1=alu2), R, W)

    def stt(eng, o, a, s, b, alu0, alu1, R, W):
        op(eng, lambda e: e.scalar_tensor_tensor(out=o, in0=a, scalar=s, in1=b, op0=alu0, op1=alu1), R, W)

    def cp(eng, o, a, R, W):
        if eng == "act":
            op("act", lambda e: e.copy(out=o, in_=a), R, W)
        else:
            op(eng, lambda e: e.tensor_copy(out=o, in_=a), R, W)

    def recip(o, a, R, W):
        op("dve", lambda e: e.reciprocal(out=o, in_=a), R, W)

    try:
        SC.phase = 'phA'
        AR.release(BASE)
        Wb = [AR.alloc([128, 1544], BF16) for _ in range(16)]
        Wst = [AR.alloc([128, 1544], F32) for _ in range(2)]
        ln1t = AR.alloc([128, 16], F32)
        zt = AR.alloc([128, 6, 2], F32)
        dma("sp", ln1t.ap, ln1.ap, W=[ln1t])
        op("pool", lambda e: e.memset(zt.ap, 0.0), [], [zt])
        cmv = cm_raw.ap[0:6].rearrange("b p t -> p b t")
        dma("pq", cmv[:, :, 0:2], zt.ap, R=[zt], W=[cm_raw.b("padl")])
        dma("pq", cmv[:, :, S + 2:S + 4], zt.ap, R=[zt], W=[cm_raw.b("padr")])
        for dc in range(16):
            st = Wst[dc % 2]
            dma("sp", st.ap, w_in.ap[dc * 128:(dc + 1) * 128, :], W=[st])
            cp("pool", Wb[dc].ap, st.ap, [st], [Wb[dc]])
        xb = [AR.alloc([128, 16, 512], F32) for _ in range(2)]
        xn = [AR.alloc([128, 16, 512], BF16) for _ in range(2)]
        sq = [AR.alloc([128, 512], BF16) for _ in range(4)]
        rt = AR.alloc([128, 512], F32)
        rstd = [AR.alloc([128, 512], F32) for _ in range(2)]
        cmo = [AR.alloc([128, 512], F32) for _ in range(3)]
        tmo = [AR.alloc([128, 392], F32) for _ in range(2)]
        p_ss = PS.tile(0, 0, [128, 512], F32)
        p_cm = [PS.tile(1 + i, 0, [128, 512], F32) for i in range(3)]
        p_tm = [PS.tile(4 + i, 0, [128, 392], F32) for i in range(2)]
        xTv = xT.ap.rearrange("(dc p) t -> p dc t", p=128)

        def A_load(tb):
            dma("sp", xb[tb % 2].ap, xTv[:, :, tb * 512:(tb + 1) * 512], W=[xb[tb % 2]])

        def A_norm(tb):
            X, XN, RS = xb[tb % 2], xn[tb % 2], rstd[tb % 2]
            for dc in range(16):
                s_ = sq[dc % 4]
                act(s_.ap, X.ap[:, dc, :], AF.Square, [X], [s_])
                mm(p_ss.ap, onesB, s_.ap, [miscb, s_], [p_ss], start=(dc == 0), stop=(dc == 15))
            act(rt.ap, p_ss.ap, AF.Sqrt, [p_ss, epst], [rt], bias=epst.ap, scale=1.0 / D)
            recip(RS.ap, rt.ap, [rt], [RS])
            for dc in range(16):
                stt("dve", XN.ap[:, dc, :], X.ap[:, dc, :], ln1t.ap[:, dc:dc + 1], RS.ap,
                    ALU.mult, ALU.mult, [X, ln1t, RS], [XN])

        def A_main(tb):
            XN = xn[tb % 2]
            for blk in range(9):
                pt = p_cm[blk % 3]
                for dc in range(16):
                    mm(pt.ap, Wb[dc].ap[:, blk * 128:(blk + 1) * 128], XN.ap[:, dc, :], [Wb[dc], XN], [pt],
                       start=(dc == 0), stop=(dc == 15))
                o = cmo[blk % 3]
                cp("act" if blk % 2 == 0 else "dve", o.ap, pt.ap, [pt], [o])
                dma("pq", cm_raw.ap[blk, :, 2 + tb * 512:2 + (tb + 1) * 512], o.ap, R=[o],
                    W=[cm_raw.b((blk, tb))])
            for t4 in range(4):
                pt = p_tm[t4 % 2]
                for dc in range(16):
                    mm(pt.ap, XN.ap[:, dc, t4 * 128:(t4 + 1) * 128], Wb[dc].ap[:, 1152:1544], [Wb[dc], XN], [pt],
                       start=(dc == 0), stop=(dc == 15))
                o = tmo[t4 % 2]
                cp("dve" if t4 % 2 == 0 else "act", o.ap, pt.ap, [pt], [o])
                r0 = tb * 512 + t4 * 128
                dma("pq", tm_raw.ap[r0:r0 + 128, :], o.ap, R=[o], W=[tm_raw.b(r0 // 128)])

        A_load(0)
        if NB > 1:
            A_load(1)
        A_norm(0)
        for tb in range(NB):
            if tb + 2 < NB:
                A_load(tb + 2)
            if tb + 1 < NB:
                A_norm(tb + 1)
            A_main(tb)

        if stop == 'A':
            raise _Stop()
        SC.phase = 'phB'
        AR.release(BASE)
        cwt = AR.alloc([128, 6, 5], F32)
        alt = AR.alloc([128, 4], F32)
        dtt = AR.alloc([128, 4], F32)
        nea = AR.alloc([128, 4], F32)
        abr = AR.alloc([128, NT, 8], F32)
        gtmp = AR.alloc([128, NT, 4], F32)
        dma("sp", cwt.ap, convw.ap, W=[cwt])
        dma("sp", alt.ap, alog.ap, W=[alt])
        dma("sp", dtt.ap, dtb.ap, W=[dtt])
        act(nea.ap, alt.ap, AF.Exp, [alt], [nea])
        ts("dve", nea.ap, nea.ap, -1.0, ALU.mult, [nea], [nea])
        for n0 in range(0, NT, 16):
            dma("sp", abr.ap[:, n0:n0 + 16, :], tm_raw.ap.rearrange("(n p) c -> p n c", p=128)[:, n0:n0 + 16, 384:392],
                R=tm_raw.all(), W=[abr])
        tt("dve", gtmp.ap, abr.ap[:, :, 0:4], dtt.ap.unsqueeze(1).to_broadcast([128, NT, 4]), ALU.add,
           [abr, dtt], [gtmp])
        act(gtmp.ap, gtmp.ap, AF.Exp, [gtmp], [gtmp])
        act(gtmp.ap, gtmp.ap, AF.Ln, [gtmp], [gtmp], bias=1.0)
        tt("dve", la_all.ap, gtmp.ap, nea.ap.unsqueeze(1).to_broadcast([128, NT, 4]), ALU.mult,
           [gtmp, nea], [la_all])
        act(be_all.ap, abr.ap[:, :, 4:8], AF.Exp, [abr], [be_all], scale=-1.0)
        ts("dve", be_all.ap, be_all.ap, 1.0, ALU.add, [be_all], [be_all])
        recip(be_all.ap, be_all.ap, [be_all], [be_all])
        ts("dve", nbe_all.ap, be_all.ap, -1.0, ALU.mult, [be_all], [nbe_all])

        if "gates_d" in dump:
            gates_d = dscr("gates_d", [128, NT, 8])
            dma("pq", gates_d.ap[:, :, 0:4], la_all.ap, R=[la_all], W=[gates_d.b(0)])
            dma("pq", gates_d.ap[:, :, 4:8], be_all.ap, R=[be_all], W=[gates_d.b(1)])
        bctx = []
        for hh in range(2):
            bctx.append(dict(
                raw=[[AR.alloc([128, 516], F32) for _ in range(3)] for _ in range(2)],
                cacc=[AR.alloc([128, 512], F32) for _ in range(3)],
                vbf=AR.alloc([128, 512], BF16), ctmp=AR.alloc([128, 512], F32),
                sqf=[AR.alloc([128, 512], F32) for _ in range(2)],
                rtb=[AR.alloc([128, 512], F32) for _ in range(2)],
                qkb=[[AR.alloc([128, 512], BF16) for _ in range(2)] for _ in range(2)],
                tmb=[[AR.alloc([128, 4, 128], BF16) for _ in range(2)] for _ in range(2)],
                p_ssb=[PS.tile(4 * hh + i, 0, [128, 512], F32) for i in range(2)],
                p_trb=[PS.tile(4 * hh + 2 + i, 0, [128, 4, 128], BF16) for i in range(2)]))

        def B_one(hh, tb):
            x_ = bctx[hh]
            raw, cacc, vbf, ctmp, sqf, rtb, qkb, tmb, p_ssb, p_trb = (x_[k_] for k_ in (
                "raw", "cacc", "vbf", "ctmp", "sqf", "rtb", "qkb", "tmb", "p_ssb", "p_trb"))
            it = tb
            rw = raw[it % 2]
            for ti in range(3):
                blk = 2 * ti + hh
                dma("sp", rw[ti].ap, cm_raw.ap[blk, :, tb * 512:tb * 512 + 516],
                    R=cm_raw.all(), W=[rw[ti]])
            for ti in range(3):
                blk = 2 * ti + hh
                acc = cacc[ti]
                eng = "dve" if ti < 2 else "pool"
                act(acc.ap, rw[ti].ap[:, 0:512], AF.Copy, [rw[ti], cwt], [acc], scale=cwt.ap[:, blk, 0:1])
                for k in range(1, 5):
                    if eng == "dve":
                        stt(eng, acc.ap, rw[ti].ap[:, k:k + 512], cwt.ap[:, blk, k:k + 1], acc.ap,
                            ALU.mult, ALU.add, [rw[ti], cwt, acc], [acc])
                    else:
                        ts("pool", ctmp.ap, rw[ti].ap[:, k:k + 512], cwt.ap[:, blk, k:k + 1], ALU.mult,
                           [rw[ti], cwt], [ctmp])
                        tt("pool", acc.ap, acc.ap, ctmp.ap, ALU.add, [acc, ctmp], [acc])
            for ti in range(2):
                acc = cacc[ti]
                act(acc.ap, acc.ap, AF.Silu, [acc], [acc])
                tt("pool", sqf[ti].ap, acc.ap, acc.ap, ALU.mult, [acc], [sqf[ti]])
                mm(p_ssb[ti].ap, onesF, sqf[ti].ap, [misc, sqf[ti]], [p_ssb[ti]])
                act(rtb[ti].ap, p_ssb[ti].ap, AF.Sqrt, [p_ssb[ti], epst], [rtb[ti]], bias=epst.ap)
                recip(rtb[ti].ap, rtb[ti].ap, [rtb[ti]], [rtb[ti]])
                ob = qkb[ti][it % 2]
                stt("dve", ob.ap, acc.ap, (128.0 ** -0.5) if ti == 0 else 1.0, rtb[ti].ap,
                    ALU.mult, ALU.mult, [acc, rtb[ti]], [ob])
                dst = dn_qT if ti == 0 else dn_kT
                dma("pq", dst.ap[hh, :, tb * 512:(tb + 1) * 512], ob.ap, R=[ob], W=[dst.b((hh, tb))])
            act(vbf.ap, cacc[2].ap, AF.Silu, [cacc[2]], [vbf])
            for ti, src in ((0, qkb[1][it % 2]), (1, vbf)):
                pt = p_trb[ti]
                for j in range(4):
                    tr(pt.ap[:, j, :], src.ap[:, j * 128:(j + 1) * 128], identB, [src, miscb], [pt])
                ob = tmb[ti][it % 2]
                cp("act" if ti == 0 else "dve", ob.ap, pt.ap, [pt], [ob])
                dst = dn_ktm if ti == 0 else dn_vtm
                dma("pq", dst.ap[hh].rearrange("(n p) d -> p n d", p=128)[:, tb * 4:(tb + 1) * 4, :], ob.ap,
                    R=[ob], W=[dst.b((hh, tb))])

        for tb in range(NB):
            interleave([lambda hh=hh, tb=tb: B_one(hh, tb) for hh in range(2)])

        if stop == 'B':
            raise _Stop()
        SC.phase = 'phC'
        AR.release(BASE)
        GT = min(8, NT)
        chains = []
        for chn in range(4):
            hh, dr = chn % 2, chn // 2
            c = dict(hh=hh, dr=dr, col=dr * 2 + hh)
            c["QG"] = [AR.alloc([128, GT * 128], BF16) for _ in range(2)]
            c["KG"] = [AR.alloc([128, GT * 128], BF16) for _ in range(2)]
            c["KtG"] = [AR.alloc([128, GT, 128], BF16) for _ in range(2)]
            c["VtG"] = [AR.alloc([128, GT, 128], BF16) for _ in range(2)]
            for nm in ("R1", "R2", "E2", "t1", "E2m", "PT0", "PT1", "N0", "N1", "M0", "M1"):
                c[nm] = AR.alloc([128, 129], F32)
            for nm in ("E1", "u"):
                c[nm] = [AR.alloc([128, 129], F32) for _ in range(2)]
            for nm in ("wT", "QKm", "kd", "TinvT"):
                c[nm] = [AR.alloc([128, 128], BF16) for _ in range(2)]
            for nm in ("vb", "kbg", "Sbf", "vnew"):
                c[nm] = AR.alloc([128, 128], BF16)
            c["S"] = AR.alloc([128, 128], F32)
            c["tmp"] = AR.alloc([128, 128], F32)
            c["o"] = [AR.alloc([128, 128], F32) for _ in range(2)]
            c["lc"] = AR.alloc([128, 2], F32)
            c["bg"] = AR.alloc([128, 1], F32)
            c["Egl"] = [AR.alloc([128, 2], F32) for _ in range(2)]
            bA, bB = 2 * chn, 2 * chn + 1
            slot = {0: 0, 1: 512, 2: 1536, 3: 1024, 4: 1536, 5: 0, 6: 512, 7: 1024}
            c["ps"] = [PS.tile(bA, slot[s], [128, 128], F32) for s in range(8)]
            c["pss"] = [PS.tile(bB, s * 512, [128, 128], F32) for s in range(4)]
            c["pG1"] = PS.tile(bA, 0, [128, 129], F32)
            c["pG2"] = PS.tile(bA, 1024, [128, 129], F32)
            op("pool", lambda e, t=c["S"]: e.memset(t.ap, 0.0), [], [c["S"]])
            op("pool", lambda e, t=c["Sbf"]: e.memset(t.ap, 0.0), [], [c["Sbf"]])
            chains.append(c)

        def C_load(c, g):
            hh, dr = c["hh"], c["dr"]
            ng = NT // GT
            gg = g if dr == 0 else ng - 1 - g
            k = g % 2
            t0 = gg * GT * 128
            dma("sp", c["QG"][k].ap, dn_qT.ap[hh, :, t0:t0 + GT * 128], R=dn_qT.all(), W=[c["QG"][k]])
            dma("sp", c["KG"][k].ap, dn_kT.ap[hh, :, t0:t0 + GT * 128], R=dn_kT.all(), W=[c["KG"][k]])
            dma("sp", c["KtG"][k].ap, dn_ktm.ap[hh].rearrange("(n p) d -> p n d", p=128)[:, gg * GT:(gg + 1) * GT, :],
                R=dn_ktm.all(), W=[c["KtG"][k]])
            dma("sp", c["VtG"][k].ap, dn_vtm.ap[hh].rearrange("(n p) d -> p n d", p=128)[:, gg * GT:(gg + 1) * GT, :],
                R=dn_vtm.all(), W=[c["VtG"][k]])

        def C_prep(c, i):
            hh, dr, col = c["hh"], c["dr"], c["col"]
            n = i if dr == 0 else NT - 1 - i
            g = i // GT
            k = g % 2
            j = n % GT
            QG, KG, KtG, VtG = c["QG"][k], c["KG"][k], c["KtG"][k], c["VtG"][k]
            qTt = QG.ap[:, j * 128:(j + 1) * 128]
            kTt = KG.ap[:, j * 128:(j + 1) * 128]
            ktm = KtG.ap[:, j, :]
            vtm = VtG.ap[:, j, :]
            la = la_all.ap[:, n, col:col + 1]
            be = be_all.ap[:, n, col:col + 1]
            nbe = nbe_all.ap[:, n, col:col + 1]
            mU, mS = (UI, SL) if dr == 0 else (LI, SU)
            ps = c["ps"]
            pb = i % 2
            R1, R2, E2, t1, E2m = c["R1"], c["R2"], c["E2"], c["t1"], c["E2m"]
            E1, u, wT, QKm, kd, Egl = c["E1"][pb], c["u"][pb], c["wT"][pb], c["QKm"][pb], c["kd"][pb], c["Egl"][pb]
            ts("dve", R1.ap[:, 0:128], mS, la, ALU.mult, [masks, la_all], [R1])
            cp("pool", R1.ap[:, 128:129], la, [la_all], [R1])
            ts("pool", R2.ap[:, 0:128], mU, la, ALU.mult, [masks, la_all], [R2])
            cp("pool", R2.ap[:, 128:129], la, [la_all], [R2])
            mm(c["pG1"].ap, mU, R1.ap, [masks, R1], [c["pG1"]])
            mm(c["pG2"].ap, mS, R2.ap, [masks, R2], [c["pG2"]])
            act(E1.ap, c["pG1"].ap, AF.Exp, [c["pG1"]], [E1])
            act(E2.ap, c["pG2"].ap, AF.Exp, [c["pG2"]], [E2])
            ts("pool", c["lc"].ap, ci.ap, la, ALU.mult, [ci, la_all], [c["lc"]])
            mm(ps[4].ap[:, 0:2], onesF, c["lc"].ap, [misc, c["lc"]], [ps[4]])
            act(Egl.ap, ps[4].ap[:, 0:2], AF.Exp, [ps[4]], [Egl])
            mm(ps[5].ap, kTt, kTt, [KG], [ps[5]])
            mm(ps[6].ap, kTt, qTt, [KG, QG], [ps[6]])
            N0, N1, M0, M1, PT0, PT1 = c["N0"], c["N1"], c["M0"], c["M1"], c["PT0"], c["PT1"]
            tt("dve", t1.ap[:, 0:128], ps[5].ap, E1.ap[:, 0:128], ALU.mult, [ps[5], E1], [t1])
            stt("dve", N0.ap[:, 0:128], t1.ap[:, 0:128], nbe, mS, ALU.mult, ALU.mult, [t1, nbe_all, masks], [N0])
            tt("pool", E2m.ap[:, 0:128], E2.ap[:, 0:128], mU, ALU.mult, [E2, masks], [E2m])
            tt("dve", QKm.ap, ps[6].ap, E2m.ap[:, 0:128], ALU.mult, [ps[6], E2m], [QKm])
            tr(ps[7].ap, N0.ap[:, 0:128], identF, [N0, misc], [ps[7]])
            cp("act", M0.ap[:, 0:128], ps[7].ap, [ps[7]], [M0])
            tt("dve", PT0.ap[:, 0:128], ps[7].ap, identF, ALU.add, [ps[7], misc], [PT0])
            Ncur, Mcur, Pcur = N0, M0, PT0
            Nnxt, Mnxt, Pnxt = N1, M1, PT1
            for lv in range(1, 6):
                mm(ps[0].ap, Mcur.ap[:, 0:128], Ncur.ap[:, 0:128], [Mcur, Ncur], [ps[0]])
                cp("act", Nnxt.ap[:, 0:128], ps[0].ap, [ps[0]], [Nnxt])
                if lv < 5:
                    mm(ps[1].ap, Ncur.ap[:, 0:128], Mcur.ap[:, 0:128], [Mcur, Ncur], [ps[1]])
                    cp("dve", Mnxt.ap[:, 0:128], ps[1].ap, [ps[1]], [Mnxt])
                mm(ps[2].ap, Nnxt.ap[:, 0:128], Pcur.ap[:, 0:128], [Nnxt, Pcur], [ps[2]])
                if lv < 5:
                    tt("dve", Pnxt.ap[:, 0:128], ps[2].ap, Pcur.ap[:, 0:128], ALU.add, [ps[2], Pcur], [Pnxt])
                else:
                    tt("dve", c["TinvT"][pb].ap, ps[2].ap, Pcur.ap[:, 0:128], ALU.add, [ps[2], Pcur], [c["TinvT"][pb]])
                Ncur, Nnxt = Nnxt, Ncur
                Mcur, Mnxt = Mnxt, Mcur
                Pcur, Pnxt = Pnxt, Pcur
            TinvT = c["TinvT"][pb]
            ts("pool", c["vb"].ap, vtm, be, ALU.mult, [VtG, be_all], [c["vb"]])
            tt("pool", c["bg"].ap, be, E1.ap[:, 128:129], ALU.mult, [be_all, E1], [c["bg"]])
            ts("pool", c["kbg"].ap, ktm, c["bg"].ap, ALU.mult, [KtG, c["bg"]], [c["kbg"]])
            ts("pool", kd.ap, ktm, E2.ap[:, 128:129], ALU.mult, [KtG, E2], [kd])
            mm(ps[3].ap, TinvT.ap, c["vb"].ap, [TinvT, c["vb"]], [ps[3]])
            cp("act", u.ap[:, 0:128], ps[3].ap, [ps[3]], [u])
            mm(ps[4].ap, c["kbg"].ap, TinvT.ap, [TinvT, c["kbg"]], [ps[4]])
            cp("act", wT.ap, ps[4].ap, [ps[4]], [wT])

        def C_scan(c, i):
            hh, dr = c["hh"], c["dr"]
            n = i if dr == 0 else NT - 1 - i
            k = (i // GT) % 2
            j = n % GT
            QG = c["QG"][k]
            qTt = QG.ap[:, j * 128:(j + 1) * 128]
            pb = i % 2
            E1, u, wT, QKm, kd, Egl = c["E1"][pb], c["u"][pb], c["wT"][pb], c["QKm"][pb], c["kd"][pb], c["Egl"][pb]
            ps = c["pss"]
            S_, Sbf, vnew, tmp, o_ = c["S"], c["Sbf"], c["vnew"], c["tmp"], c["o"][pb]
            for cc in ((0, 1) if dr == 0 else (1, 0)):
                pc = slice(cc * 64, cc * 64 + 64)
                mm(ps[0].ap, wT.ap, Sbf.ap, [wT, Sbf], [ps[0]])
                mm(ps[1].ap, qTt, Sbf.ap, [QG, Sbf], [ps[1]])
                tt("dve", vnew.ap[pc, :], u.ap[pc, 0:128], ps[0].ap[pc, :], ALU.subtract, [u, ps[0]], [vnew])
                mm(ps[2].ap, QKm.ap[pc, :], vnew.ap[pc, :], [QKm, vnew], [ps[2]])
                mm(ps[3].ap, kd.ap[pc, :], vnew.ap[pc, :], [kd, vnew], [ps[3]])
                act(tmp.ap[pc, :], ps[1].ap[pc, :], AF.Copy, [ps[1], E1], [tmp], scale=E1.ap[pc, 128:129])
                tt("dve", o_.ap[pc, :], tmp.ap[pc, :], ps[2].ap[pc, :], ALU.add, [tmp, ps[2]], [o_])
                stt("dve", S_.ap, S_.ap, Egl.ap[:, cc:cc + 1], ps[3].ap, ALU.mult, ALU.add, [S_, Egl, ps[3]], [S_])
                cp("act", Sbf.ap, S_.ap, [S_], [Sbf])
            dma("pq", dn_o.ap[chn_idx(c), n * 128:(n + 1) * 128, :], o_.ap, R=[o_], W=[dn_o.b((chn_idx(c), n))])

        def chn_idx(c):
            return c["dr"] * 2 + c["hh"]

        for c in chains:
            C_load(c, 0)
        for i in range(NT + 1):
            if i % GT == 1 and (i // GT + 1) * GT < NT:
                for c in chains:
                    C_load(c, i // GT + 1)
            gens = []
            if i >= 1:
                gens += [lambda c=c, i=i: C_scan(c, i - 1) for c in chains]
            if i < NT:
                gens += [lambda c=c, i=i: C_prep(c, i) for c in chains]
            interleave(gens)

        if "gates_c" in dump:
            gates_c = dscr("gates_c", [128, NT, 12])
            dma("pq", gates_c.ap[:, :, 0:4], la_all.ap, R=[la_all], W=[gates_c.b(0)])
            dma("pq", gates_c.ap[:, :, 4:8], be_all.ap, R=[be_all], W=[gates_c.b(1)])
            dma("pq", gates_c.ap[:, :, 8:12], nbe_all.ap, R=[nbe_all], W=[gates_c.b(2)])
        if stop == 'C':
            raise _Stop()
        SC.phase = 'phD'
        AR.release(BASE)
        dnwt = AR.alloc([128, 128], F32)
        dma("sp", dnwt.ap, dnw.ap, W=[dnwt])
        DG = min(4, NT)
        of_ = [AR.alloc([128, DG, 128], F32) for _ in range(2)]
        ob_ = [AR.alloc([128, DG, 128], F32) for _ in range(2)]
        zg = [AR.alloc([128, DG, 128], F32) for _ in range(2)]
        dtmp = [dict(osum=AR.alloc([128, 128], F32), osq=AR.alloc([128, 128], F32), ms=AR.alloc([128, 1], F32),
                     rs=AR.alloc([128, 1], F32), sz=AR.alloc([128, 128], F32), on=AR.alloc([128, 128], F32),
                     resb=AR.alloc([128, 128], BF16)) for _ in range(DG)]
        mo = [[AR.alloc([128, 128], BF16) for _ in range(DG)] for _ in range(2)]
        p_d = [PS.tile(i, 0, [128, 128], BF16) for i in range(DG)]
        it = 0
        for hh in range(2):
            for g in range(NT // DG):
                k = it % 2
                r0 = g * DG * 128
                dma("sp", of_[k].ap, dn_o.ap[hh].rearrange("(n p) d -> p n d", p=128)[:, g * DG:(g + 1) * DG, :],
                    R=dn_o.all(), W=[of_[k]])
                dma("sp", ob_[k].ap, dn_o.ap[2 + hh].rearrange("(n p) d -> p n d", p=128)[:, g * DG:(g + 1) * DG, :],
                    R=dn_o.all(), W=[ob_[k]])
                dma("sp", zg[k].ap, tm_raw.ap.rearrange("(n p) c -> p n c", p=128)[:, g * DG:(g + 1) * DG, hh * 128:(hh + 1) * 128],
                    R=tm_raw.all(), W=[zg[k]])
                def D_one(j, k=k, hh=hh, g=g, r0=r0):
                    t_ = dtmp[j]
                    osum, osq, ms, rs_, sz, on, resb = (t_[x] for x in ("osum", "osq", "ms", "rs", "sz", "on", "resb"))
                    tt("dve", osum.ap, of_[k].ap[:, j, :], ob_[k].ap[:, j, :], ALU.add, [of_[k], ob_[k]], [osum])
                    op("pool", lambda e: e.memset(ms.ap, 0.0), [], [ms])
                    act(osq.ap, osum.ap, AF.Square, [osum], [osq, ms], accum=ms.ap)
                    act(rs_.ap, ms.ap, AF.Sqrt, [ms, epst], [rs_], bias=epst.ap, scale=1.0 / 128)
                    recip(rs_.ap, rs_.ap, [rs_], [rs_])
                    stt("dve", on.ap, osum.ap, rs_.ap, dnwt.ap, ALU.mult, ALU.mult, [osum, rs_, dnwt], [on])
                    act(sz.ap, zg[k].ap[:, j, :], AF.Silu, [zg[k]], [sz])
                    tt("pool", resb.ap, on.ap, sz.ap, ALU.mult, [on, sz], [resb])
                    pt = p_d[j]
                    tr(pt.ap, resb.ap, identB, [resb, miscb], [pt])
                    m_ = mo[k][j]
                    cp("act", m_.ap, pt.ap, [pt], [m_])
                    c0 = r0 % TQ + j * 128
                    dma("pq", mixT_loc.ap[r0 // TQ, hh * 128:(hh + 1) * 128, c0:c0 + 128], m_.ap, R=[m_],
                        W=[mixT_loc.b(("dn", hh, g, j))])
                interleave([lambda j=j: D_one(j) for j in range(DG)])
                it += 1

        if stop == 'D':
            raise _Stop()
        SC.phase = 'phE'
        AR.release(BASE)
        QT = [AR.alloc([128, S], BF16) for _ in range(2)]
        KT = AR.alloc([128, S], BF16)
        Vt = AR.alloc([128, NT, 128], BF16)
        qw = AR.alloc([128, 1], F32)
        kw_ = AR.alloc([128, 1], F32)
        nrow = AR.alloc([128, 2, 128], F32)
        mx2 = AR.alloc([128, 2], F32)
        nbias = AR.alloc([128, 1], F32)
        dma("sp", qw.ap, qnw.ap, W=[qw])
        dma("sp", kw_.ap, knw.ap, W=[kw_])
        dma("sp", nrow.ap[:, 0, :], qnrow.ap, W=[nrow])
        dma("sp", nrow.ap[:, 1, :], knrow.ap, W=[nrow])
        tt("dve", nrow.ap, nrow.ap, nrow.ap, ALU.mult, [nrow], [nrow])
        op("dve", lambda e: e.tensor_reduce(out=mx2.ap, in_=nrow.ap, axis=AX.X, op=ALU.max), [nrow], [mx2])
        tt("dve", nbias.ap, mx2.ap[:, 0:1], mx2.ap[:, 1:2], ALU.mult, [mx2], [nbias])
        act(nbias.ap, nbias.ap, AF.Sqrt, [nbias], [nbias])
        ts("dve", nbias.ap, nbias.ap, -math.sqrt(128.0), ALU.mult, [nbias], [nbias])
        EM = AR.mark()
        araw = [[AR.alloc([128, 512], F32) for _ in range(3)] for _ in range(2)]
        cst = [[AR.alloc([128, 512], F32) for _ in range(2)] for _ in range(2)]
        asq = AR.alloc([128, 512], F32)
        art = AR.alloc([128, 512], F32)
        axn = AR.alloc([128, 512], F32)
        at1 = AR.alloc([128, 512], F32)
        at2 = AR.alloc([128, 512], F32)
        vst = [AR.alloc([128, 4, 128], F32) for _ in range(2)]
        p_as = PS.tile(0, 0, [128, 512], F32)
        p_ar = PS.tile(1, 0, [128, 512], F32)
        for tb in range(NB):
            k = tb % 2
            for ti in range(3):
                dma("sp", araw[k][ti].ap, cm_raw.ap[6 + ti, :, 2 + tb * 512:2 + (tb + 1) * 512], R=cm_raw.all(),
                    W=[araw[k][ti]])
            dma("sp", cst[k][0].ap, cosT.ap[:, tb * 512:(tb + 1) * 512], W=[cst[k][0]])
            dma("sp", cst[k][1].ap, sinT.ap[:, tb * 512:(tb + 1) * 512], W=[cst[k][1]])
            dma("sp", vst[k].ap, tm_raw.ap.rearrange("(n p) c -> p n c", p=128)[:, tb * 4:(tb + 1) * 4, 256:384],
                R=tm_raw.all(), W=[vst[k]])
            cp("pool", Vt.ap[:, tb * 4:(tb + 1) * 4, :], vst[k].ap, [vst[k]], [Vt])
            for ti in range(3):
                x_ = araw[k][ti]
                w_ = qw if ti < 2 else kw_
                dst = QT[ti] if ti < 2 else KT
                tt("pool", asq.ap, x_.ap, x_.ap, ALU.mult, [x_], [asq])
                mm(p_as.ap, onesF, asq.ap, [misc, asq], [p_as])
                act(art.ap, p_as.ap, AF.Sqrt, [p_as, epst], [art], bias=epst.ap, scale=1.0 / 128)
                recip(art.ap, art.ap, [art], [art])
                stt("dve", axn.ap, x_.ap, w_.ap, art.ap, ALU.mult, ALU.mult, [x_, w_, art], [axn])
                mm(p_ar.ap, rotF, axn.ap, [misc, axn], [p_ar])
                tt("pool", at1.ap, axn.ap, cst[k][0].ap, ALU.mult, [axn, cst[k][0]], [at1])
                tt("dve", at2.ap, p_ar.ap, cst[k][1].ap, ALU.mult, [p_ar, cst[k][1]], [at2])
                tt("dve", dst.ap[:, tb * 512:(tb + 1) * 512], at1.ap, at2.ap, ALU.add, [at1, at2], [dst])
        AR.release(EM)
        NPT = 4
        ptile = [AR.alloc([128, 512], BF16) for _ in range(NPT)]
        racc = [AR.alloc([128, 512], F32) for _ in range(2)]
        rsum = AR.alloc([128, 512], F32)
        rinv = AR.alloc([128, 512], F32)
        otn = [AR.alloc([128, 512], BF16) for _ in range(2)]
        p_st = [PS.tile(i, 0, [128, 512], F32) for i in range(4)]
        p_ot = [PS.tile(4 + i, 0, [128, 512], F32) for i in range(2)]
        p_rs = PS.tile(6, 0, [128, 512], F32)
        sc_att = 128.0 ** -0.5
        qi = 0
        for h in range(2):
            for qt in range(NB):
                pot = p_ot[qi % 2]
                qs = QT[h].ap[:, qt * 512:(qt + 1) * 512]
                for kt in range(NT):
                    pst = p_st[kt % 4]
                    mm(pst.ap, KT.ap[:, kt * 128:(kt + 1) * 128], qs, [KT, QT[h]], [pst])
                    pt_ = ptile[kt % NPT]
                    act(pt_.ap, pst.ap, AF.Exp, [pst, nbias], [pt_], bias=nbias.ap, scale=sc_att)
                    mm(pot.ap, Vt.ap[:, kt, :], pt_.ap, [Vt, pt_], [pot], start=(kt == 0), stop=(kt == NT - 1))
                    ra = racc[kt % 2]
                    eng = "dve" if kt % 2 == 0 else "pool"
                    if kt < 2:
                        cp(eng, ra.ap, pt_.ap, [pt_], [ra])
                    else:
                        tt(eng, ra.ap, ra.ap, pt_.ap, ALU.add, [ra, pt_], [ra])
                if NT > 1:
                    tt("dve", rsum.ap, racc[0].ap, racc[1].ap, ALU.add, [racc[0], racc[1]], [rsum])
                else:
                    cp("dve", rsum.ap, racc[0].ap, [racc[0]], [rsum])
                mm(p_rs.ap, onesF, rsum.ap, [misc, rsum], [p_rs])
                recip(rinv.ap, p_rs.ap, [p_rs], [rinv])
                ot = otn[qi % 2]
                tt("dve", ot.ap, pot.ap, rinv.ap, ALU.mult, [pot, rinv], [ot])
                dma("pq", mixT_loc.ap[(qt * 512) // TQ, 256 + h * 128:256 + (h + 1) * 128,
                                      (qt * 512) % TQ:(qt * 512) % TQ + 512], ot.ap, R=[ot],
                    W=[mixT_loc.b(("att", h, qt))])
                qi += 1

        if stop == 'E':
            raise _Stop()
        SC.phase = 'phF'
        for q in range(NQ):
            SC.cc(lambda e, q=q: e.collective_compute("AllGather", ALU.bypass, replica_groups=GRP,
                                                      ins=[mixT_loc.ap[q]], outs=[mixT_full.ap[q]]),
                  R=mixT_loc.all(), W=[mixT_full.b()])

        if stop == 'F':
            raise _Stop()
        SC.phase = 'phG'
        AR.release(BASE)
        WO = [AR.alloc([128, D], BF16) for _ in range(16)]
        wst = [AR.alloc([128, D], F32) for _ in range(2)]
        for cc in range(16):
            st = wst[cc % 2]
            dma("sp", st.ap, w_out.ap[cc * 128:(cc + 1) * 128, :], W=[st])
            cp("pool", WO[cc].ap, st.ap, [st], [WO[cc]])
        ln2t = AR.alloc([128, D], F32)
        wrt = AR.alloc([128, 16, NE], F32)
        dma("sp", ln2t.ap, ln2bc.ap, W=[ln2t])
        dma("sp", wrt.ap, wr.ap, W=[wrt])
        MB = min(512, TPC)
        mixb = [AR.alloc([128, 16, MB], BF16) for _ in range(2)]
        xt_ = [AR.alloc([128, D], F32) for _ in range(2)]
        x1t = [AR.alloc([128, D], F32) for _ in range(2)]
        h2f = AR.alloc([128, D], F32)
        h2b = [AR.alloc([128, D], BF16) for _ in range(2)]
        h2T = AR.alloc([128, 16, 128], F32)
        gsq = AR.alloc([128, D], BF16)
        gms = AR.alloc([128, 1], F32)
        grs = AR.alloc([128, 1], F32)
        lmx = AR.alloc([128, 1], F32)
        lsum = AR.alloc([128, 1], F32)
        lex = AR.alloc([128, NE], F32)
        afft = [AR.alloc([128, NE], F32) for _ in range(2)]
        p_y = [PS.tile(i, 0, [128, 512], F32) for i in range(4)]
        p_t = [PS.tile(4 + i, 0, [128, 4, 128], F32) for i in range(2)]
        p_l = PS.tile(6, 0, [128, NE], F32)
        mfv = mixT_full.ap.rearrange("q (cc p) t -> q cc p t", p=128)

        def G_loadmix(mbi):
            def f(e, mbi=mbi):
                qd = dynval(e, TPC // TQ, (mbi * MB) // TQ)
                o_ = (mbi * MB) % TQ
                return e.dma_start(out=mixb[mbi % 2].ap,
                                   in_=mfv[bass.ds(qd, 1), :, :, o_:o_ + MB].rearrange("q cc p t -> p (q cc) t"))
            op("sp", f, R=[mixT_full.b()], W=[mixb[mbi % 2]], dma=True)

        G_loadmix(0)
        for i in range(NTC):
            mbi, mo_ = (i * 128) // MB, (i * 128) % MB
            if mo_ == 0 and (mbi + 1) * MB < TPC:
                G_loadmix(mbi + 1)
            k = i % 2
            dma("sp", xt_[k].ap, xtok.ap[i * 128:(i + 1) * 128, :], W=[xt_[k]])
            mb_ = mixb[mbi % 2]
            for db in range(4):
                for cc in range(16):
                    mm(p_y[db].ap, mb_.ap[:, cc, mo_:mo_ + 128], WO[cc].ap[:, db * 512:(db + 1) * 512], [mb_, WO[cc]],
                       [p_y[db]], start=(cc == 0), stop=(cc == 15))
            for db in range(4):
                tt("dve", x1t[k].ap[:, db * 512:(db + 1) * 512], p_y[db].ap, xt_[k].ap[:, db * 512:(db + 1) * 512],
                   ALU.add, [p_y[db], xt_[k]], [x1t[k]])
            dma("pq", x1_loc.ap[i * 128:(i + 1) * 128, :], x1t[k].ap, R=[x1t[k]], W=[x1_loc.b(i)])
            op("pool", lambda e: e.memset(gms.ap, 0.0), [], [gms])
            act(gsq.ap, x1t[k].ap, AF.Square, [x1t[k]], [gsq, gms], accum=gms.ap)
            act(grs.ap, gms.ap, AF.Sqrt, [gms, epst], [grs], bias=epst.ap, scale=1.0 / D)
            recip(grs.ap, grs.ap, [grs], [grs])
            stt("dve", h2f.ap, x1t[k].ap, grs.ap, ln2t.ap, ALU.mult, ALU.mult, [x1t[k], grs, ln2t], [h2f])
            cp("pool", h2b[k].ap, h2f.ap, [h2f], [h2b[k]])
            dma("pq", h2_loc.ap[i * 128:(i + 1) * 128, :], h2b[k].ap, R=[h2b[k]], W=[h2_loc.b(i)])
            for q4 in range(4):
                pt = p_t[q4 % 2]
                for j in range(4):
                    dc = q4 * 4 + j
                    tr(pt.ap[:, j, :], h2f.ap[:, dc * 128:(dc + 1) * 128], identF, [h2f, misc], [pt])
                cp("act" if q4 % 2 == 0 else "dve", h2T.ap[:, q4 * 4:(q4 + 1) * 4, :], pt.ap, [pt], [h2T])
            for dc in range(16):
                mm(p_l.ap, h2T.ap[:, dc, :], wrt.ap[:, dc, :], [h2T, wrt], [p_l], start=(dc == 0), stop=(dc == 15))
            op("dve", lambda e: e.tensor_reduce(out=lmx.ap, in_=p_l.ap, axis=AX.X, op=ALU.max), [p_l], [lmx])
            ts("dve", lmx.ap, lmx.ap, -1.0, ALU.mult, [lmx], [lmx])
            op("pool", lambda e: e.memset(lsum.ap, 0.0), [], [lsum])
            act(lex.ap, p_l.ap, AF.Exp, [p_l, lmx], [lex, lsum], bias=lmx.ap, accum=lsum.ap)
            recip(lsum.ap, lsum.ap, [lsum], [lsum])
            ts("dve", afft[k].ap, lex.ap, lsum.ap, ALU.mult, [lex, lsum], [afft[k]])
            dma("pq", aff_loc.ap[i * 128:(i + 1) * 128, :], afft[k].ap, R=[afft[k]], W=[aff_loc.b(i)])

        if stop == 'G':
            raise _Stop()
        SC.phase = 'phH'
        SC.cc(lambda e: e.collective_compute("AllGather", ALU.bypass, replica_groups=GRP,
                                             ins=[aff_loc.ap], outs=[aff_full.ap]),
              R=aff_loc.all(), W=[aff_full.b()])
        for q in range(TPC // HQ):
            SC.cc(lambda e, q=q: e.collective_compute("AllGather", ALU.bypass, replica_groups=GRP,
                                                      ins=[h2_loc.ap[q * HQ:(q + 1) * HQ, :]],
                                                      outs=[h2_full.ap[q * 4 * HQ:(q + 1) * 4 * HQ, :]]),
                  R=h2_loc.all(), W=[h2_full.b()])

        if stop == 'H':
            raise _Stop()
        SC.phase = 'phI'
        AR.release(BASE)
        A3 = AR.alloc([128, NT, NE], F32)
        AE = AR.alloc([128, NE, NT], F32)
        cmpt = AR.alloc([128, NE, NT], F32)
        selb = AR.alloc([128, NE, NT], BF16)
        cum = AR.alloc([128, NE, NT], F32)
        tta = AR.alloc([128, NE, NT], F32)
        ttb = AR.alloc([128, NE, NT], F32)
        pos = AR.alloc([128, NE, NT], F32)
        posi = AR.alloc([128, NE, NT], I32)
        posgi = AR.alloc([128, NE, NT], I32)
        gsf = AR.alloc([128, NE, NT], F32)
        lo = AR.alloc([128, NE, 1], F32)
        hi = AR.alloc([128, NE, 1], F32)
        mid = AR.alloc([128, NE, 1], F32)
        dlt = AR.alloc([128, NE, 1], F32)
        ge = AR.alloc([128, NE, 1], F32)
        cntp = AR.alloc([128, NE], F32)
        ecap = AR.alloc([128, NE, 1], F32)
        p_c = PS.tile(0, 0, [128, NE], F32)
        dma("sp", ecap.ap[:, :, 0], cecap.ap, W=[ecap])
        for n0 in range(0, NT, 16):
            dma("sp", A3.ap[:, n0:n0 + 16, :], aff_full.ap.rearrange("(n p) e -> p n e", p=128)[:, n0:n0 + 16, :],
                R=[aff_full.b()], W=[A3])
        cp("dve", AE.ap, A3.ap.rearrange("p n e -> p e n"), [A3], [AE])
        op("dve", lambda e: e.memset(lo.ap, 0.0), [], [lo])
        op("dve", lambda e: e.memset(hi.ap, 1.0), [], [hi])
        NESH = [128, NE, NT]
        for itr in range(34):
            tt("dve", mid.ap, lo.ap, hi.ap, ALU.add, [lo, hi], [mid])
            ts("dve", mid.ap, mid.ap, 0.5, ALU.mult, [mid], [mid])
            tt("dve", cmpt.ap, AE.ap, mid.ap.to_broadcast(NESH), ALU.is_gt, [AE, mid], [cmpt])
            op("dve", lambda e: e.tensor_reduce(out=cntp.ap, in_=cmpt.ap, axis=AX.X, op=ALU.add), [cmpt], [cntp])
            mm(p_c.ap, onesF, cntp.ap, [misc, cntp], [p_c])
            ts("dve", ge.ap[:, :, 0], p_c.ap, float(CAP) - 0.5, ALU.is_gt, [p_c], [ge])
            tt("dve", dlt.ap, mid.ap, lo.ap, ALU.subtract, [mid, lo], [dlt])
            tt("dve", dlt.ap, dlt.ap, ge.ap, ALU.mult, [dlt, ge], [dlt])
            tt("dve", lo.ap, lo.ap, dlt.ap, ALU.add, [lo, dlt], [lo])
            tt("dve", dlt.ap, hi.ap, mid.ap, ALU.subtract, [hi, mid], [dlt])
            tt("dve", dlt.ap, dlt.ap, ge.ap, ALU.mult, [dlt, ge], [dlt])
            tt("dve", hi.ap, mid.ap, dlt.ap, ALU.add, [mid, dlt], [hi])
        tt("dve", cmpt.ap, AE.ap, lo.ap.to_broadcast(NESH), ALU.is_gt, [AE, lo], [cmpt])
        cp("pool", selb.ap, cmpt.ap, [cmpt], [selb])
        tt("pool", gsf.ap, AE.ap, cmpt.ap, ALU.mult, [AE, cmpt], [gsf])
        dma("pq", gs_d.ap, gsf.ap, R=[gsf], W=[gs_d.b()])
        selv = selb.ap.rearrange("p e n -> p (e n)")
        cumv = cum.ap.rearrange("p e n -> p (e n)")
        ttav = tta.ap.rearrange("p e n -> p (e n)")
        NEN = NE * NT
        CW = min(512, NEN)
        pcs = [PS.tile(1 + i, 0, [128, CW], F32) for i in range(2)]
        for j in range(NEN // CW):
            pt = pcs[j % 2]
            mm(pt.ap, UFb, selv[:, j * CW:(j + 1) * CW], [miscb, selb], [pt])
            cp("act", cumv[:, j * CW:(j + 1) * CW], pt.ap, [pt], [cum])
            pt2 = pcs[(j + 1) % 2]
            mm(pt2.ap, onesB, selv[:, j * CW:(j + 1) * CW], [miscb, selb], [pt2])
            cp("dve", ttav[:, j * CW:(j + 1) * CW], pt2.ap, [pt2], [tta])
        cp("pool", pos.ap, tta.ap, [tta], [pos])
        src, dst = tta, ttb
        sh = 1
        while sh < NT:
            cp("dve", dst.ap[:, :, 0:sh], src.ap[:, :, 0:sh], [src], [dst])
            tt("dve", dst.ap[:, :, sh:NT], src.ap[:, :, sh:NT], src.ap[:, :, 0:NT - sh], ALU.add, [src], [dst])
            src, dst = dst, src
            sh *= 2
        tt("dve", dst.ap, src.ap, pos.ap, ALU.subtract, [src, pos], [dst])
        tt("dve", pos.ap, cum.ap, dst.ap, ALU.add, [cum, dst], [pos])
        ts("dve", pos.ap, pos.ap, -1.0 - BIG, ALU.add, [pos], [pos])
        tt("dve", pos.ap, pos.ap, cmpt.ap, ALU.mult, [pos, cmpt], [pos])
        ts("dve", pos.ap, pos.ap, BIG, ALU.add, [pos], [pos])
        cp("dve", posi.ap, pos.ap, [pos], [posi])
        op("dve", lambda e: e.memset(tta.ap, 0.0), [], [tta])
        for kq in range(1, CAP // HQ):
            ts("dve", ttb.ap, pos.ap, kq * HQ - 0.5, ALU.is_gt, [pos], [ttb])
            tt("dve", tta.ap, tta.ap, ttb.ap, ALU.add, [tta, ttb], [tta])
        stt("dve", pos.ap, tta.ap, 3.0 * HQ, pos.ap, ALU.mult, ALU.add, [tta, pos], [pos])
        tt("dve", pos.ap, pos.ap, ecap.ap.to_broadcast(NESH), ALU.add, [pos, ecap], [pos])
        cp("dve", posgi.ap, pos.ap, [pos], [posgi])
        dma("pq", pos_d.ap, posi.ap, R=[posi], W=[pos_d.b()])
        dma("pq", posg_d.ap, posgi.ap, R=[posgi], W=[posg_d.b()])

        if stop == 'I':
            raise _Stop()
        SC.phase = 'phJ'
        AR.release(BASE)
        mypos = AR.alloc([128, 4, NT], I32)

        def J_ld(e):
            return e.dma_start(out=mypos.ap, in_=pos_d.ap[:, bass.ds(dynval(e, 4, 0), 4), :])
        op("pq", J_ld, R=[pos_d.b()], W=[mypos], dma=True)
        hrow = [AR.alloc([128, D], BF16) for _ in range(3)]
        for n in range(NT):
            hr = hrow[n % 3]
            rr_, tl0 = n // NTC, (n % NTC) * 128
            row0 = (tl0 // HQ) * 4 * HQ + rr_ * HQ + tl0 % HQ
            dma("sp", hr.ap, h2_full.ap[row0:row0 + 128, :], R=[h2_full.b()], W=[hr])
            for j in range(4):
                def f(e, j=j, n=n, hr=hr):
                    return e.indirect_dma_start(out=xs[j].ap, out_offset=bass.IndirectOffsetOnAxis(ap=mypos.ap[:, j, n:n + 1], axis=0),
                                                in_=hr.ap, in_offset=None, bounds_check=bcreg(e, CAP - 1), oob_is_err=False)
                op("pq", f, R=[hr, mypos], W=[xs[j].b(n)], dma=True)

        if stop == 'J':
            raise _Stop()
        SC.phase = 'phK'
        AR.release(BASE)
        xsT = AR.alloc([128, 16, CAP], BF16)
        hmT = AR.alloc([128, 16, CAP], BF16)
        KM = AR.mark()
        p_x = [PS.tile(i, 0, [128, 4, 128], BF16) for i in range(2)]
        p_a = [PS.tile(2 + i, 0, [128, TBK], F32) for i in range(2)]
        p_u = [PS.tile(4 + i, 0, [128, TBK], F32) for i in range(2)]
        p_yk = [PS.tile(6 + i, 0, [128, 256], F32) for i in range(2)]
        for j in range(4):
            AR.release(KM)
            xrow = [AR.alloc([128, D], BF16) for _ in range(2)]
            for t_ in range(CAP // 128):
                xr = xrow[t_ % 2]
                dma("sp", xr.ap, xs[j].ap[t_ * 128:(t_ + 1) * 128, :], R=xs[j].all(), W=[xr])
                for q4 in range(4):
                    pt = p_x[q4 % 2]
                    for jj in range(4):
                        dc = q4 * 4 + jj
                        tr(pt.ap[:, jj, :], xr.ap[:, dc * 128:(dc + 1) * 128], identB, [xr, miscb], [pt])
                    cp("act" if q4 % 2 == 0 else "dve", xsT.ap[:, q4 * 4:(q4 + 1) * 4, t_ * 128:(t_ + 1) * 128], pt.ap,
                       [pt], [xsT])
            AR.release(KM)
            gst = [AR.alloc([128, 16, 128], F32) for _ in range(2)]
            ust = [AR.alloc([128, 16, 128], F32) for _ in range(2)]
            gbf = [AR.alloc([128, 16, 128], BF16) for _ in range(2)]
            ubf = [AR.alloc([128, 16, 128], BF16) for _ in range(2)]
            sa = [AR.alloc([128, TBK], F32) for _ in range(2)]
            wgv = wg.ap[j].rearrange("(dc p) f -> p dc f", p=128)
            wuv = wu.ap[j].rearrange("(dc p) f -> p dc f", p=128)

            def K_ldw(fc):
                k = fc % 2
                dma("sp", gst[k].ap, wgv[:, :, fc * 128:(fc + 1) * 128], W=[gst[k]])
                dma("sp", ust[k].ap, wuv[:, :, fc * 128:(fc + 1) * 128], W=[ust[k]])
                cp("pool", gbf[k].ap, gst[k].ap, [gst[k]], [gbf[k]])
                cp("pool", ubf[k].ap, ust[k].ap, [ust[k]], [ubf[k]])
            K_ldw(0)
            ii = 0
            for fc in range(16):
                if fc + 1 < 16:
                    K_ldw(fc + 1)
                k = fc % 2
                for tb in range(CAP // TBK):
                    pa, pu = p_a[ii % 2], p_u[ii % 2]
                    for dc in range(16):
                        mm(pa.ap, gbf[k].ap[:, dc, :], xsT.ap[:, dc, tb * TBK:(tb + 1) * TBK], [gbf[k], xsT], [pa],
                           start=(dc == 0), stop=(dc == 15))
                    for dc in range(16):
                        mm(pu.ap, ubf[k].ap[:, dc, :], xsT.ap[:, dc, tb * TBK:(tb + 1) * TBK], [ubf[k], xsT], [pu],
                           start=(dc == 0), stop=(dc == 15))
                    s_ = sa[ii % 2]
                    act(s_.ap, pa.ap, AF.Silu, [pa], [s_])
                    tt("dve", hmT.ap[:, fc, tb * TBK:(tb + 1) * TBK], s_.ap, pu.ap, ALU.mult, [s_, pu], [hmT])
                    ii += 1
            AR.release(KM)
            dst_ = [AR.alloc([128, 16, 256], F32) for _ in range(2)]
            dbf = [AR.alloc([128, 16, 256], BF16) for _ in range(2)]
            yst = [AR.alloc([128, 256], BF16) for _ in range(3)]
            wdv = wd.ap[j].rearrange("(fc p) d -> p fc d", p=128)

            def K_ldd(db):
                k = db % 2
                dma("sp", dst_[k].ap, wdv[:, :, db * 256:(db + 1) * 256], W=[dst_[k]])
                cp("pool", dbf[k].ap, dst_[k].ap, [dst_[k]], [dbf[k]])
            K_ldd(0)
            ii = 0
            for db in range(8):
                if db + 1 < 8:
                    K_ldd(db + 1)
                k = db % 2
                for t_ in range(CAP // 128):
                    py = p_yk[ii % 2]
                    for fc in range(16):
                        mm(py.ap, hmT.ap[:, fc, t_ * 128:(t_ + 1) * 128], dbf[k].ap[:, fc, :], [hmT, dbf[k]], [py],
                           start=(fc == 0), stop=(fc == 15))
                    ys = yst[ii % 3]
                    cp("act" if ii % 2 == 0 else "dve", ys.ap, py.ap, [py], [ys])
                    dma("pq", y_loc.ap[j * CAP + t_ * 128:j * CAP + (t_ + 1) * 128, db * 256:(db + 1) * 256], ys.ap,
                        R=[ys], W=[y_loc.b((j, db, t_))])
                    ii += 1

        if stop == 'K':
            raise _Stop()
        SC.phase = 'phL'
        for q in range(4 * CAP // HQ):
            SC.cc(lambda e, q=q: e.collective_compute("AllGather", ALU.bypass, replica_groups=GRP,
                                                      ins=[y_loc.ap[q * HQ:(q + 1) * HQ, :]],
                                                      outs=[y_full.ap[q * 4 * HQ:(q + 1) * 4 * HQ, :]]),
                  R=y_loc.all(), W=[y_full.b()])

        if stop == 'L':
            raise _Stop()
        SC.phase = 'phM'
        AR.release(BASE)
        cpos = AR.alloc([128, NE, NTC], I32)
        cgs = AR.alloc([128, NE, NTC], F32)

        def M_ld1(e):
            return e.dma_start(out=cpos.ap, in_=posg_d.ap[:, :, bass.ds(dynval(e, NTC, 0), NTC)])

        def M_ld2(e):
            return e.dma_start(out=cgs.ap, in_=gs_d.ap[:, :, bass.ds(dynval(e, NTC, 0), NTC)])
        op("pq", M_ld1, R=[posg_d.b()], W=[cpos], dma=True)
        op("pq", M_ld2, R=[gs_d.b()], W=[cgs], dma=True)
        NGB = 32
        gbuf = [AR.alloc([128, D], BF16) for _ in range(NGB)]
        for gb in gbuf:
            op("pool", lambda e, gb=gb: e.memset(gb.ap, 0.0), [], [gb])
        accm = [AR.alloc([128, D], F32) for _ in range(2)]
        gi = 0
        for i in range(NTC):
            a0 = accm[i % 2]
            dma("sp", a0.ap, x1_loc.ap[i * 128:(i + 1) * 128, :], R=[x1_loc.b(i)], W=[a0])
            for e_ in range(NE):
                gb = gbuf[gi % NGB]
                gi += 1

                def f(e, gb=gb, e_=e_, i=i):
                    return e.indirect_dma_start(out=gb.ap, out_offset=None, in_=y_full.ap,
                                                in_offset=bass.IndirectOffsetOnAxis(ap=cpos.ap[:, e_, i:i + 1], axis=0),
                                                bounds_check=bcreg(e, NE * CAP - 1), oob_is_err=False)
                op("pq", f, R=[y_full.b(), cpos], W=[gb], dma=True)
                stt("dve", a0.ap, gb.ap, cgs.ap[:, e_, i:i + 1], a0.ap, ALU.mult, ALU.add, [gb, cgs, a0], [a0])
            dma("sp", out.ap[i * 128:(i + 1) * 128, :], a0.ap, R=[a0], W=[out.b(i)])

    except _Stop:
        pass
    for nm in dump:
        src = scr[nm]
        dbg = nc.dram_tensor('dbg_' + nm, list(src.ap.shape), src.ap.dtype, kind='ExternalOutput').ap()
        dma('sp', dbg, src.ap, R=src.all(), W=[DT(None).b()])
    SC.emit()
    return nc, SC


def _consts(S):
    CAP = 2 * S // NE
    p = np.arange(128)[:, None]
    f = np.arange(128)[None, :]
    same = (p // 64) == (f // 64)
    cmask = np.stack([(p >= f) & same, (p > f) & same, (p <= f) & same, (p < f) & same], 1).astype(np.float32)
    ident = np.eye(128, dtype=np.float32)
    rot = np.zeros((128, 128), np.float32)
    for m in range(128):
        blk = m // 32
        if blk % 2 == 0:
            rot[m + 32, m] = -1.0
        else:
            rot[m - 32, m] = 1.0
    uf = (p <= f).astype(np.float32)
    ones = np.ones((128, 128), np.float32)
    cmisc = np.stack([ident, rot, uf, ones], 1).astype(np.float32)
    cci = np.stack([(np.arange(128) < 64), (np.arange(128) >= 64)], 1).astype(np.float32)
    cecap = np.tile(np.array([(e % 4) * 4 * CAP + (e // 4) * 256 for e in range(NE)], np.float32)[None, :], (128, 1))
    t = np.arange(S)
    row = (t // 64).astype(np.float32)
    col = (t % 64).astype(np.float32)
    nf = 32
    inv = (10000.0 ** (-np.arange(nf, dtype=np.float32) / nf)).astype(np.float32)
    ang_r = row[:, None] * inv
    ang_c = col[:, None] * inv
    ang = np.concatenate([ang_r, ang_r, ang_c, ang_c], -1).astype(np.float32)
    cosT = np.ascontiguousarray(np.cos(ang).T.astype(np.float32))
    sinT = np.ascontiguousarray(np.sin(ang).T.astype(np.float32))
    return dict(cmask=np.ascontiguousarray(cmask), cmisc=np.ascontiguousarray(cmisc), cci=cci, cecap=cecap,
                cosT=cosT, sinT=sinT)


_CACHE = {}


def kernel(x, ln1_w, w_in, conv_w, a_log, dt_bias, dn_norm_w, q_norm_w, k_norm_w,
           w_out, ln2_w, w_router, w_gate, w_up, w_down):
    x = np.asarray(x, np.float32)
    B, S, _ = x.shape
    assert B == 2
    TPC = S // 4
    f32 = lambda a: np.ascontiguousarray(np.asarray(a, np.float32))
    ln1_w, w_in, conv_w, a_log, dt_bias = f32(ln1_w)[0], np.asarray(w_in, np.float32)[0], f32(conv_w)[0], f32(a_log)[0], f32(dt_bias)[0]
    dn_norm_w, q_norm_w, k_norm_w = f32(dn_norm_w)[0], f32(q_norm_w)[0], f32(k_norm_w)[0]
    w_out, ln2_w, w_router = np.asarray(w_out, np.float32)[0], f32(ln2_w)[0], f32(w_router)[0]
    w_gate, w_up, w_down = np.asarray(w_gate, np.float32)[0], np.asarray(w_up, np.float32)[0], np.asarray(w_down, np.float32)[0]
    if S not in _CACHE:
        _CACHE[S] = build(S)
    nc, _ = _CACHE[S]
    cst = _consts(S)
    xTs = [np.ascontiguousarray(x[b].T) for b in range(B)]
    perm = []
    for r in range(4):
        for blk in (2 * r, 2 * r + 1):
            perm.extend(range(blk * 128, (blk + 1) * 128))
        for blk in (2 * r, 2 * r + 1):
            perm.extend(range(1024 + blk * 128, 1024 + (blk + 1) * 128))
    w_out_p = np.ascontiguousarray(w_out[np.array(perm)])
    ln1_l = np.ascontiguousarray(ln1_w.reshape(16, 128).T)
    ln2bc = np.ascontiguousarray(np.tile(ln2_w[None, :], (128, 1)))
    wr_l = np.ascontiguousarray(w_router.reshape(16, 128, NE).transpose(1, 0, 2))
    dnw = np.ascontiguousarray(np.tile(dn_norm_w[None, :], (128, 1)))
    qnrow = np.ascontiguousarray(np.tile(q_norm_w[None, :], (128, 1)))
    knrow = np.ascontiguousarray(np.tile(k_norm_w[None, :], (128, 1)))
    in_maps = []
    for c in range(8):
        b, r = c // 4, c % 4
        kv = r // 2
        h0, h1 = 2 * r, 2 * r + 1
        sl = lambda off, h: list(range(off + h * 128, off + (h + 1) * 128))
        cols = (sl(0, h0) + sl(0, h1) + sl(1024, h0) + sl(1024, h1) + sl(2048, h0) + sl(2048, h1)
                + sl(4128, h0) + sl(4128, h1) + sl(5152, kv)
                + sl(3072, h0) + sl(3072, h1) + sl(5408, kv)
                + [4096 + h0, 4096 + h1, 4104 + h0, 4104 + h1, 4112 + h0, 4112 + h1, 4120 + h0, 4120 + h1])
        cols = np.array(cols)
        cch = np.array(sl(0, h0) + sl(0, h1) + sl(1024, h0) + sl(1024, h1) + sl(2048, h0) + sl(2048, h1))
        convw = np.ascontiguousarray(conv_w[:, cch].reshape(5, 6, 128).transpose(2, 1, 0))
        sel4 = lambda a: np.ascontiguousarray(np.tile(np.array([a[0, h0], a[0, h1], a[1, h0], a[1, h1]], np.float32)[None, :], (128, 1)))
        m = dict(
            xT=xTs[b], xtok=np.ascontiguousarray(x[b, r * TPC:(r + 1) * TPC]),
            w_in=np.ascontiguousarray(w_in[:, cols]), ln1=ln1_l, convw=convw,
            alog=sel4(a_log), dtb=sel4(dt_bias), dnw=dnw,
            qnw=np.ascontiguousarray(q_norm_w[:, None]), knw=np.ascontiguousarray(k_norm_w[:, None]),
            qnrow=qnrow, knrow=knrow, w_out=w_out_p, ln2bc=ln2bc, wr=wr_l,
            wg=np.ascontiguousarray(w_gate[4 * r:4 * r + 4]), wu=np.ascontiguousarray(w_up[4 * r:4 * r + 4]),
            wd=np.ascontiguousarray(w_down[4 * r:4 * r + 4]),
        )
        m.update(cst)
        in_maps.append(m)
    res = run_bass_kernel_spmd(nc, in_maps, core_ids=list(range(8)))
    y = np.empty((B, S, D), np.float32)
    for c in range(8):
        b, r = c // 4, c % 4
        y[b, r * TPC:(r + 1) * TPC] = res.results[c]["out"]
    return y
```

```python
import math
import numpy as np
import concourse.bass as bass
import concourse.mybir as mybir
from concourse.bass_utils import run_bass_kernel_spmd

F32 = mybir.dt.float32
BF16 = mybir.dt.bfloat16
I32 = mybir.dt.int32
U8 = mybir.dt.uint8
ALU = mybir.AluOpType
AF = mybir.ActivationFunctionType
AX = mybir.AxisListType
DSIZE = {F32: 4, BF16: 2, I32: 4, U8: 1}

D = 2048
EPS = 1e-6
NE = 16
BIG = float(1 << 20)


class Buf:
    __slots__ = ("lw", "rd")

    def __init__(self):
        self.lw = None
        self.rd = []


class Tile:
    __slots__ = ("ap", "bufs", "excl")

    def __init__(self, ap, bufs, excl=False):
        self.ap = ap
        self.bufs = bufs
        self.excl = excl

    def __getitem__(self, k):
        return self.ap[k]


class DT:
    def __init__(self, ap):
        self.ap = ap
        self._b = {}

    def b(self, key=0):
        if key not in self._b:
            self._b[key] = Tile(None, [Buf()])
        return self._b[key]

    def all(self):
        return list(self._b.values())

    def __getitem__(self, k):
        return self.ap[k]


class Arena:
    def __init__(self, nc, name, nbytes, gran=512):
        self.ap = nc.alloc_sbuf_tensor(name, [128, nbytes], U8).ap()
        self.gran = gran
        self.nbytes = nbytes
        self.bufs = [Buf() for _ in range((nbytes + gran - 1) // gran)]
        self.off = 0

    def alloc(self, shape, dt):
        n = 1
        for s in shape[1:]:
            n *= s
        nb = n * DSIZE[dt]
        al = 512 if nb >= 512 else 32
        off = (self.off + al - 1) // al * al
        assert off + nb <= self.nbytes, f"SBUF arena overflow {off}+{nb}"
        self.off = off + nb
        v = self.ap[0:shape[0], off:off + nb].bitcast(dt)
        if len(shape) == 3:
            v = v.rearrange("p (a b) -> p a b", a=shape[1])
        elif len(shape) == 4:
            v = v.rearrange("p (a b c) -> p a b c", a=shape[1], b=shape[2])
        g0, g1 = off // self.gran, (off + nb - 1) // self.gran
        return Tile(v, self.bufs[g0:g1 + 1])

    def mark(self):
        return self.off

    def release(self, m):
        self.off = m


class PsumArena:
    def __init__(self, nc):
        self.banks = [nc.alloc_psum_tensor(f"pbank{i}", [128, 512], F32).ap() for i in range(8)]
        self.bufs = [[Buf()] for _ in range(8)]

    def tile(self, bank, off_bytes, shape, dt):
        n = 1
        for s in shape[1:]:
            n *= s
        nb = n * DSIZE[dt]
        assert off_bytes % 4 == 0 and off_bytes + nb <= 2048
        v = self.banks[bank][0:shape[0], off_bytes // 4:(off_bytes + nb + 3) // 4]
        if dt != F32:
            v = v.bitcast(dt)
        if len(shape) == 3:
            v = v.rearrange("p (a b) -> p a b", a=shape[1])
        return Tile(v, self.bufs[bank], excl=True)


class Sched:
    NDMA = 16
    scopes = False
    LIM = 3000

    def __init__(self, nc):
        self.nc = nc
        self.ops = []
        self.phase = "init"
        self.phases = []

    @staticmethod
    def _flat(ts):
        out = []
        for t in ts:
            if isinstance(t, Buf):
                out.append(t)
            else:
                out.extend(t.bufs)
        return out

    def op(self, eng, fn, R=(), W=(), dma=False):
        R = list(R)
        W = list(W) + [t for t in R if isinstance(t, Tile) and t.excl]
        self.ops.append((eng, fn, self._flat(R), self._flat(W), dma))
        self.phases.append(self.phase)

    def dma(self, q, out, in_, R=(), W=(), **kw):
        self.op(q, lambda e: e.dma_start(out=out, in_=in_, **kw), R, W, dma=True)

    def cc(self, fn, R=(), W=()):
        self.op("pq", fn, R, W, dma="cc")

    def emit(self):
        nc = self.nc
        ops = self.ops
        n = len(ops)
        stream_of = {"pe": "pe", "act": "act", "dve": "dve", "pool": "pool",
                     "sp": "sp", "pq": "pool", "aq": "act"}
        handles = {"pe": nc.tensor, "act": nc.scalar, "dve": nc.vector, "pool": nc.gpsimd,
                   "sp": nc.sync}
        deps = [None] * n
        signal = [False] * n
        for i, (eng, fn, reads, writes, dma) in enumerate(ops):
            d = set()
            raw = set()
            for b in reads:
                if b.lw is not None:
                    d.add(b.lw)
                    raw.add(b.lw)
            for b in writes:
                if b.lw is not None:
                    d.add(b.lw)
                d.update(b.rd)
            for b in writes:
                b.lw = i
                b.rd = []
            if not dma:
                st_i = stream_of[eng]
                for b in reads:
                    if b.lw != i:
                        b.rd = [j for j in b.rd if ops[j][4] or stream_of[ops[j][0]] != st_i]
                        b.rd.append(i)
            else:
                for b in reads:
                    if b.lw != i:
                        b.rd.append(i)
            d.discard(i)
            st = stream_of[eng]
            dd = []
            for j in d:
                ej, _, _, _, dj = ops[j]
                if dj or stream_of[ej] != st or (j in raw and st != "pe"):
                    dd.append(j)
                    signal[j] = True
            deps[i] = dd
            if dma:
                signal[i] = True
        streams = ("pe", "act", "dve", "pool", "sp")
        sems = {s: nc.alloc_semaphore(name=f"s_{s}_0") for s in streams}
        epoch = {s: 0 for s in streams}
        used_q = {eng for (eng, _, _, _, dma) in ops if dma and dma != "cc"}
        dsems = {q: [nc.alloc_semaphore(name=f"d_{q}{k}") for k in range(self.NDMA)]
                 for q in ("sp", "pq", "aq") if q in used_q}
        cnt = {s: 0 for s in streams}
        dcnt = {q: 0 for q in dsems}
        semval = [None] * n
        ccsem = nc.alloc_semaphore(name="ccsem")
        ccn = 0
        for i, (eng, fn, reads, writes, dma) in enumerate(ops):
            if dma == "cc":
                ccn += 1
                semval[i] = (ccsem, ccn, ("cc", 0, 0))
            elif dma:
                k = dcnt[eng] % self.NDMA
                semval[i] = (dsems[eng][k], 16 * (dcnt[eng] // self.NDMA + 1), ("d", eng, k))
                dcnt[eng] += 1
            elif signal[i]:
                st = stream_of[eng]
                if cnt[st] >= self.LIM:
                    epoch[st] += 1
                    sems[st] = nc.alloc_semaphore(name=f"s_{st}_{epoch[st]}")
                    cnt[st] = 0
                cnt[st] += 1
                semval[i] = (sems[st], cnt[st], ("c", st, epoch[st]))
        seen = {s: {} for s in streams}
        dma_hist = {q: [] for q in dsems}
        for i, (eng, fn, reads, writes, dma) in enumerate(ops):
            st = stream_of[eng]
            h = handles[st]
            waits = {}
            for j in deps[i]:
                sem, v, key = semval[j]
                if waits.get(key, (None, 0))[1] < v:
                    waits[key] = (sem, v)
            if dma and dma != "cc":
                hist = dma_hist[eng]
                if len(hist) >= self.NDMA:
                    sem, v, key = semval[hist[-self.NDMA]]
                    if waits.get(key, (None, 0))[1] < v:
                        waits[key] = (sem, v)
                hist.append(i)
            for key, (sem, v) in waits.items():
                if seen[st].get(key, 0) < v:
                    h.wait_ge(sem, v)
                    seen[st][key] = v
            if self.scopes and (i == 0 or self.phases[i] != self.phases[i - 1]):
                if i > 0:
                    nc.pop_named_scope(self.phases[i - 1])
                nc.push_named_scope(self.phases[i])
            inst = fn(h)
            if semval[i] is not None:
                sem, v, key = semval[i]
                if dma == "cc":
                    inst.then_inc(sem)
                else:
                    inst.then_inc(sem, 16 if dma else 1)
        if self.scopes and n:
            nc.pop_named_scope(self.phases[n - 1])
        last = {}
        for i in range(n):
            if semval[i] is not None:
                sem, v, key = semval[i]
                if last.get(key, (None, 0))[1] < v:
                    last[key] = (sem, v)
        for key, (sem, v) in last.items():
            if seen["sp"].get(key, 0) < v:
                nc.sync.wait_ge(sem, v)
        self.stats = dict(n_ops=n, cnt=cnt, dcnt=dcnt)


class _Stop(Exception):
    pass


def build(S, stop=None, dump=()):
    NT = S // 128
    NB = S // 512
    TPC = S // 4
    NTC = TPC // 128
    CAP = 2 * S // NE
    TBK = min(512, CAP)
    GRP = [[0, 1, 2, 3], [4, 5, 6, 7]]
    TQ = min(1024, TPC)
    NQ = S // TQ
    HQ = 256

    nc = bass.Bass("TRN2", target_bir_lowering=False)
    SC = Sched(nc)
    op, dma = SC.op, SC.dma

    def din(name, shape, dt=F32):
        return DT(nc.dram_tensor(name, shape, dt, kind="ExternalInput").ap())

    scr = {}

    def dscr(name, shape, dt=F32):
        t = DT(nc.dram_tensor(name, shape, dt).ap())
        scr[name] = t
        return t

    xT = din("xT", [D, S])
    xtok = din("xtok", [TPC, D])
    w_in = din("w_in", [D, 1544])
    ln1 = din("ln1", [128, 16])
    convw = din("convw", [128, 6, 5])
    alog = din("alog", [128, 4])
    dtb = din("dtb", [128, 4])
    dnw = din("dnw", [128, 128])
    qnw = din("qnw", [128, 1])
    knw = din("knw", [128, 1])
    qnrow = din("qnrow", [128, 128])
    knrow = din("knrow", [128, 128])
    cosT = din("cosT", [128, S])
    sinT = din("sinT", [128, S])
    w_out = din("w_out", [D, D])
    ln2bc = din("ln2bc", [128, D])
    wr = din("wr", [128, 16, NE])
    wg = din("wg", [4, D, D])
    wu = din("wu", [4, D, D])
    wd = din("wd", [4, D, D])
    cmask = din("cmask", [128, 4, 128])
    cmisc = din("cmisc", [128, 4, 128])
    cci = din("cci", [128, 2])
    cecap = din("cecap", [128, NE])
    out = DT(nc.dram_tensor("out", [TPC, D], F32, kind="ExternalOutput").ap())

    cm_raw = dscr("cm_raw", [9, 128, S + 4])
    tm_raw = dscr("tm_raw", [S, 392])
    dn_qT = dscr("dn_qT", [2, 128, S], BF16)
    dn_kT = dscr("dn_kT", [2, 128, S], BF16)
    dn_ktm = dscr("dn_ktm", [2, S, 128], BF16)
    dn_vtm = dscr("dn_vtm", [2, S, 128], BF16)
    dn_o = dscr("dn_o", [4, S, 128])
    mixT_loc = dscr("mixT_loc", [NQ, 512, TQ], BF16)
    mixT_full = dscr("mixT_full", [NQ, 2048, TQ], BF16)
    x1_loc = dscr("x1_loc", [TPC, D])
    h2_loc = dscr("h2_loc", [TPC, D], BF16)
    h2_full = dscr("h2_full", [S, D], BF16)
    aff_loc = dscr("aff_loc", [TPC, NE])
    aff_full = dscr("aff_full", [S, NE])
    pos_d = dscr("pos_d", [128, NE, NT], I32)
    posg_d = dscr("posg_d", [128, NE, NT], I32)
    gs_d = dscr("gs_d", [128, NE, NT])
    xs = [dscr(f"xs{j}", [CAP, D], BF16) for j in range(4)]
    y_loc = dscr("y_loc", [4 * CAP, D], BF16)
    y_full = dscr("y_full", [NE * CAP, D], BF16)

    AR = Arena(nc, "arena", 200 * 1024)
    PS = PsumArena(nc)

    masks = AR.alloc([128, 4, 128], F32)
    misc = AR.alloc([128, 4, 128], F32)
    miscb = AR.alloc([128, 4, 128], BF16)
    ci = AR.alloc([128, 2], F32)
    epst = AR.alloc([128, 1], F32)
    la_all = AR.alloc([128, NT, 4], F32)
    be_all = AR.alloc([128, NT, 4], F32)
    nbe_all = AR.alloc([128, NT, 4], F32)
    dma("sp", masks.ap, cmask.ap, W=[masks])
    dma("sp", misc.ap, cmisc.ap, W=[misc])
    dma("sp", ci.ap, cci.ap, W=[ci])
    op("pool", lambda e: e.tensor_copy(out=miscb.ap, in_=misc.ap), [misc], [miscb])
    op("pool", lambda e: e.memset(epst.ap, EPS), [], [epst])
    LI, SL, UI, SU = (masks.ap[:, i, :] for i in range(4))
    identF, rotF, UFf, onesF = (misc.ap[:, i, :] for i in range(4))
    identB, UFb, onesB = miscb.ap[:, 0, :], miscb.ap[:, 2, :], miscb.ap[:, 3, :]
    BASE = AR.mark()

    _regs = {}

    def bcreg(e, val):
        if val not in _regs:
            _regs[val] = e.to_reg(val)
        return _regs[val]

    _dyn = {}

    def dynval(e, mul, add):
        ek = id(e)
        if ("r", ek) not in _dyn:
            _dyn[("r", ek)] = e.snap(e.partition_id() % 4)
        key = (ek, mul, add)
        if key not in _dyn:
            _dyn[key] = e.snap(_dyn[("r", ek)] * mul + add)
        return _dyn[key]

    def interleave(gens):
        lists = []
        for g in gens:
            so, sp_ = SC.ops, SC.phases
            SC.ops, SC.phases = [], []
            g()
            lists.append((SC.ops, SC.phases))
            SC.ops, SC.phases = so, sp_
        idx = [0] * len(lists)
        more = True
        while more:
            more = False
            for k, (lo, lp) in enumerate(lists):
                if idx[k] < len(lo):
                    SC.ops.append(lo[idx[k]])
                    SC.phases.append(lp[idx[k]])
                    idx[k] += 1
                    more = True

    def mm(o, lhsT, rhs, R, W, start=True, stop=True):
        op("pe", lambda e: e.matmul(o, lhsT=lhsT, rhs=rhs, start=start, stop=stop), R, W)

    def tr(o, in_, ident, R, W):
        op("pe", lambda e: e.transpose(out=o, in_=in_, identity=ident), R, W)

    def act(o, in_, func, R, W, bias=None, scale=None, accum=None):
        kw = {}
        if bias is not None:
            kw["bias"] = bias
        if scale is not None:
            kw["scale"] = scale
        if accum is not None:
            kw["accum_out"] = accum
        op("act", lambda e: e.activation(out=o, in_=in_, func=func, **kw), R, W)

    def tt(eng, o, a, b, alu, R, W):
        op(eng, lambda e: e.tensor_tensor(out=o, in0=a, in1=b, op=alu), R, W)

    def ts(eng, o, a, s1, alu1, R, W, s2=None, alu2=None):
        if alu2 is None:
            op(eng, lambda e: e.tensor_scalar(out=o, in0=a, scalar1=s1, scalar2=None, op0=alu1), R, W)
        else:
            op(eng, lambda e: e.tensor_scalar(out=o, in0=a, scalar1=s1, scalar2=s2, op0=alu1, op1=alu2), R, W)

    def stt(eng, o, a, s, b, alu0, alu1, R, W):
        op(eng, lambda e: e.scalar_tensor_tensor(out=o, in0=a, scalar=s, in1=b, op0=alu0, op1=alu1), R, W)

    def cp(eng, o, a, R, W):
        if eng == "act":
            op("act", lambda e: e.copy(out=o, in_=a), R, W)
        else:
            op(eng, lambda e: e.tensor_copy(out=o, in_=a), R, W)

    def recip(o, a, R, W):
        op("dve", lambda e: e.reciprocal(out=o, in_=a), R, W)

    try:
        SC.phase = 'phA'
        AR.release(BASE)
        Wb = [AR.alloc([128, 1544], BF16) for _ in range(16)]
        Wst = [AR.alloc([128, 1544], F32) for _ in range(2)]
        ln1t = AR.alloc([128, 16], F32)
        zt = AR.alloc([128, 6, 2], F32)
        dma("sp", ln1t.ap, ln1.ap, W=[ln1t])
        op("pool", lambda e: e.memset(zt.ap, 0.0), [], [zt])
        cmv = cm_raw.ap[0:6].rearrange("b p t -> p b t")
        dma("pq", cmv[:, :, 0:2], zt.ap, R=[zt], W=[cm_raw.b("padl")])
        dma("pq", cmv[:, :, S + 2:S + 4], zt.ap, R=[zt], W=[cm_raw.b("padr")])
        for dc in range(16):
            st = Wst[dc % 2]
            dma("sp", st.ap, w_in.ap[dc * 128:(dc + 1) * 128, :], W=[st])
            cp("pool", Wb[dc].ap, st.ap, [st], [Wb[dc]])
        xb = [AR.alloc([128, 16, 512], F32) for _ in range(2)]
        xn = [AR.alloc([128, 16, 512], BF16) for _ in range(2)]
        sq = [AR.alloc([128, 512], BF16) for _ in range(4)]
        rt = AR.alloc([128, 512], F32)
        rstd = [AR.alloc([128, 512], F32) for _ in range(2)]
        cmo = [AR.alloc([128, 512], F32) for _ in range(3)]
        tmo = [AR.alloc([128, 392], F32) for _ in range(2)]
        p_ss = PS.tile(0, 0, [128, 512], F32)
        p_cm = [PS.tile(1 + i, 0, [128, 512], F32) for i in range(3)]
        p_tm = [PS.tile(4 + i, 0, [128, 392], F32) for i in range(2)]
        xTv = xT.ap.rearrange("(dc p) t -> p dc t", p=128)

        def A_load(tb):
            dma("sp", xb[tb % 2].ap, xTv[:, :, tb * 512:(tb + 1) * 512], W=[xb[tb % 2]])

        def A_norm(tb):
            X, XN, RS = xb[tb % 2], xn[tb % 2], rstd[tb % 2]
            for dc in range(16):
                s_ = sq[dc % 4]
                act(s_.ap, X.ap[:, dc, :], AF.Square, [X], [s_])
                mm(p_ss.ap, onesB, s_.ap, [miscb, s_], [p_ss], start=(dc == 0), stop=(dc == 15))
            act(rt.ap, p_ss.ap, AF.Sqrt, [p_ss, epst], [rt], bias=epst.ap, scale=1.0 / D)
            recip(RS.ap, rt.ap, [rt], [RS])
            for dc in range(16):
                stt("dve", XN.ap[:, dc, :], X.ap[:, dc, :], ln1t.ap[:, dc:dc + 1], RS.ap,
                    ALU.mult, ALU.mult, [X, ln1t, RS], [XN])

        def A_main(tb):
            XN = xn[tb % 2]
            for blk in range(9):
                pt = p_cm[blk % 3]
                for dc in range(16):
                    mm(pt.ap, Wb[dc].ap[:, blk * 128:(blk + 1) * 128], XN.ap[:, dc, :], [Wb[dc], XN], [pt],
                       start=(dc == 0), stop=(dc == 15))
                o = cmo[blk % 3]
                cp("act" if blk % 2 == 0 else "dve", o.ap, pt.ap, [pt], [o])
                dma("pq", cm_raw.ap[blk, :, 2 + tb * 512:2 + (tb + 1) * 512], o.ap, R=[o],
                    W=[cm_raw.b((blk, tb))])
            for t4 in range(4):
                pt = p_tm[t4 % 2]
                for dc in range(16):
                    mm(pt.ap, XN.ap[:, dc, t4 * 128:(t4 + 1) * 128], Wb[dc].ap[:, 1152:1544], [Wb[dc], XN], [pt],
                       start=(dc == 0), stop=(dc == 15))
                o = tmo[t4 % 2]
                cp("dve" if t4 % 2 == 0 else "act", o.ap, pt.ap, [pt], [o])
                r0 = tb * 512 + t4 * 128
                dma("pq", tm_raw.ap[r0:r0 + 128, :], o.ap, R=[o], W=[tm_raw.b(r0 // 128)])

        A_load(0)
        if NB > 1:
            A_load(1)
        A_norm(0)
        for tb in range(NB):
            if tb + 2 < NB:
                A_load(tb + 2)
            if tb + 1 < NB:
                A_norm(tb + 1)
            A_main(tb)

        if stop == 'A':
            raise _Stop()
        SC.phase = 'phB'
        AR.release(BASE)
        cwt = AR.alloc([128, 6, 5], F32)
        alt = AR.alloc([128, 4], F32)
        dtt = AR.alloc([128, 4], F32)
        nea = AR.alloc([128, 4], F32)
        abr = AR.alloc([128, NT, 8], F32)
        gtmp = AR.alloc([128, NT, 4], F32)
        dma("sp", cwt.ap, convw.ap, W=[cwt])
        dma("sp", alt.ap, alog.ap, W=[alt])
        dma("sp", dtt.ap, dtb.ap, W=[dtt])
        act(nea.ap, alt.ap, AF.Exp, [alt], [nea])
        ts("dve", nea.ap, nea.ap, -1.0, ALU.mult, [nea], [nea])
        for n0 in range(0, NT, 16):
            dma("sp", abr.ap[:, n0:n0 + 16, :], tm_raw.ap.rearrange("(n p) c -> p n c", p=128)[:, n0:n0 + 16, 384:392],
                R=tm_raw.all(), W=[abr])
        tt("dve", gtmp.ap, abr.ap[:, :, 0:4], dtt.ap.unsqueeze(1).to_broadcast([128, NT, 4]), ALU.add,
           [abr, dtt], [gtmp])
        act(gtmp.ap, gtmp.ap, AF.Exp, [gtmp], [gtmp])
        act(gtmp.ap, gtmp.ap, AF.Ln, [gtmp], [gtmp], bias=1.0)
        tt("dve", la_all.ap, gtmp.ap, nea.ap.unsqueeze(1).to_broadcast([128, NT, 4]), ALU.mult,
           [gtmp, nea], [la_all])
        act(be_all.ap, abr.ap[:, :, 4:8], AF.Exp, [abr], [be_all], scale=-1.0)
        ts("dve", be_all.ap, be_all.ap, 1.0, ALU.add, [be_all], [be_all])
        recip(be_all.ap, be_all.ap, [be_all], [be_all])
        ts("dve", nbe_all.ap, be_all.ap, -1.0, ALU.mult, [be_all], [nbe_all])

        if "gates_d" in dump:
            gates_d = dscr("gates_d", [128, NT, 8])
            dma("pq", gates_d.ap[:, :, 0:4], la_all.ap, R=[la_all], W=[gates_d.b(0)])
            dma("pq", gates_d.ap[:, :, 4:8], be_all.ap, R=[be_all], W=[gates_d.b(1)])
        bctx = []
        for hh in range(2):
            bctx.append(dict(
                raw=[[AR.alloc([128, 516], F32) for _ in range(3)] for _ in range(2)],
                cacc=[AR.alloc([128, 512], F32) for _ in range(3)],
                vbf=AR.alloc([128, 512], BF16), ctmp=AR.alloc([128, 512], F32),
                sqf=[AR.alloc([128, 512], F32) for _ in range(2)],
                rtb=[AR.alloc([128, 512], F32) for _ in range(2)],
                qkb=[[AR.alloc([128, 512], BF16) for _ in range(2)] for _ in range(2)],
                tmb=[[AR.alloc([128, 4, 128], BF16) for _ in range(2)] for _ in range(2)],
                p_ssb=[PS.tile(4 * hh + i, 0, [128, 512], F32) for i in range(2)],
                p_trb=[PS.tile(4 * hh + 2 + i, 0, [128, 4, 128], BF16) for i in range(2)]))

        def B_one(hh, tb):
            x_ = bctx[hh]
            raw, cacc, vbf, ctmp, sqf, rtb, qkb, tmb, p_ssb, p_trb = (x_[k_] for k_ in (
                "raw", "cacc", "vbf", "ctmp", "sqf", "rtb", "qkb", "tmb", "p_ssb", "p_trb"))
            it = tb
            rw = raw[it % 2]
            for ti in range(3):
                blk = 2 * ti + hh
                dma("sp", rw[ti].ap, cm_raw.ap[blk, :, tb * 512:tb * 512 + 516],
                    R=cm_raw.all(), W=[rw[ti]])
            for ti in range(3):
                blk = 2 * ti + hh
                acc = cacc[ti]
                eng = "dve" if ti < 2 else "pool"
                act(acc.ap, rw[ti].ap[:, 0:512], AF.Copy, [rw[ti], cwt], [acc], scale=cwt.ap[:, blk, 0:1])
                for k in range(1, 5):
                    if eng == "dve":
                        stt(eng, acc.ap, rw[ti].ap[:, k:k + 512], cwt.ap[:, blk, k:k + 1], acc.ap,
                            ALU.mult, ALU.add, [rw[ti], cwt, acc], [acc])
                    else:
                        ts("pool", ctmp.ap, rw[ti].ap[:, k:k + 512], cwt.ap[:, blk, k:k + 1], ALU.mult,
                           [rw[ti], cwt], [ctmp])
                        tt("pool", acc.ap, acc.ap, ctmp.ap, ALU.add, [acc, ctmp], [acc])
            for ti in range(2):
                acc = cacc[ti]
                act(acc.ap, acc.ap, AF.Silu, [acc], [acc])
                tt("pool", sqf[ti].ap, acc.ap, acc.ap, ALU.mult, [acc], [sqf[ti]])
                mm(p_ssb[ti].ap, onesF, sqf[ti].ap, [misc, sqf[ti]], [p_ssb[ti]])
            for ti in range(2):
                acc = cacc[ti]
                act(rtb[ti].ap, p_ssb[ti].ap, AF.Sqrt, [p_ssb[ti], epst], [rtb[ti]], bias=epst.ap)
                recip(rtb[ti].ap, rtb[ti].ap, [rtb[ti]], [rtb[ti]])
                ob = qkb[ti][it % 2]
                stt("dve", ob.ap, acc.ap, (128.0 ** -0.5) if ti == 0 else 1.0, rtb[ti].ap,
                    ALU.mult, ALU.mult, [acc, rtb[ti]], [ob])
                dst = dn_qT if ti == 0 else dn_kT
                dma("pq", dst.ap[hh, :, tb * 512:(tb + 1) * 512], ob.ap, R=[ob], W=[dst.b((hh, tb))])
            act(vbf.ap, cacc[2].ap, AF.Silu, [cacc[2]], [vbf])
            for ti, src in ((0, qkb[1][it % 2]), (1, vbf)):
                pt = p_trb[ti]
                for j in range(4):
                    tr(pt.ap[:, j, :], src.ap[:, j * 128:(j + 1) * 128], identB, [src, miscb], [pt])
                ob = tmb[ti][it % 2]
                cp("act" if ti == 0 else "dve", ob.ap, pt.ap, [pt], [ob])
                dst = dn_ktm if ti == 0 else dn_vtm
                dma("pq", dst.ap[hh].rearrange("(n p) d -> p n d", p=128)[:, tb * 4:(tb + 1) * 4, :], ob.ap,
                    R=[ob], W=[dst.b((hh, tb))])

        for tb in range(NB):
            interleave([lambda hh=hh, tb=tb: B_one(hh, tb) for hh in range(2)])

        if stop == 'B':
            raise _Stop()
        SC.phase = 'phC'
        AR.release(BASE)
        GT = min(8, NT)
        chains = []
        for chn in range(4):
            hh, dr = chn % 2, chn // 2
            c = dict(hh=hh, dr=dr, col=dr * 2 + hh)
            c["QG"] = [AR.alloc([128, GT * 128], BF16) for _ in range(2)]
            c["KG"] = [AR.alloc([128, GT * 128], BF16) for _ in range(2)]
            c["KtG"] = [AR.alloc([128, GT, 128], BF16) for _ in range(2)]
            c["VtG"] = [AR.alloc([128, GT, 128], BF16) for _ in range(2)]
            for nm in ("R1", "R2", "E2", "t1", "E2m", "PT0", "PT1", "N0", "N1", "M0", "M1"):
                c[nm] = AR.alloc([128, 129], F32)
            for nm in ("E1", "u"):
                c[nm] = [AR.alloc([128, 129], F32) for _ in range(2)]
            for nm in ("wT", "QKm", "kd", "TinvT"):
                c[nm] = [AR.alloc([128, 128], BF16) for _ in range(2)]
            for nm in ("vb", "kbg", "Sbf", "vnew"):
                c[nm] = AR.alloc([128, 128], BF16)
            c["S"] = AR.alloc([128, 128], F32)
            c["tmp"] = AR.alloc([128, 128], F32)
            c["o"] = [AR.alloc([128, 128], F32) for _ in range(2)]
            c["lc"] = AR.alloc([128, 2], F32)
            c["bg"] = AR.alloc([128, 1], F32)
            c["Egl"] = [AR.alloc([128, 2], F32) for _ in range(2)]
            bA, bB = 2 * chn, 2 * chn + 1
            slot = {0: 0, 1: 512, 2: 1536, 3: 1024, 4: 1536, 5: 0, 6: 512, 7: 1024}
            c["ps"] = [PS.tile(bA, slot[s], [128, 128], F32) for s in range(8)]
            c["pss"] = [PS.tile(bB, s * 512, [128, 128], F32) for s in range(4)]
            c["pG1"] = PS.tile(bA, 0, [128, 129], F32)
            c["pG2"] = PS.tile(bA, 1024, [128, 129], F32)
            op("pool", lambda e, t=c["S"]: e.memset(t.ap, 0.0), [], [c["S"]])
            op("pool", lambda e, t=c["Sbf"]: e.memset(t.ap, 0.0), [], [c["Sbf"]])
            chains.append(c)

        def C_load(c, g):
            hh, dr = c["hh"], c["dr"]
            ng = NT // GT
            gg = g if dr == 0 else ng - 1 - g
            k = g % 2
            t0 = gg * GT * 128
            dma("sp", c["QG"][k].ap, dn_qT.ap[hh, :, t0:t0 + GT * 128], R=dn_qT.all(), W=[c["QG"][k]])
            dma("sp", c["KG"][k].ap, dn_kT.ap[hh, :, t0:t0 + GT * 128], R=dn_kT.all(), W=[c["KG"][k]])
            dma("sp", c["KtG"][k].ap, dn_ktm.ap[hh].rearrange("(n p) d -> p n d", p=128)[:, gg * GT:(gg + 1) * GT, :],
                R=dn_ktm.all(), W=[c["KtG"][k]])
            dma("sp", c["VtG"][k].ap, dn_vtm.ap[hh].rearrange("(n p) d -> p n d", p=128)[:, gg * GT:(gg + 1) * GT, :],
                R=dn_vtm.all(), W=[c["VtG"][k]])

        def C_prep(c, i):
            hh, dr, col = c["hh"], c["dr"], c["col"]
            n = i if dr == 0 else NT - 1 - i
            g = i // GT
            k = g % 2
            j = n % GT
            QG, KG, KtG, VtG = c["QG"][k], c["KG"][k], c["KtG"][k], c["VtG"][k]
            qTt = QG.ap[:, j * 128:(j + 1) * 128]
            kTt = KG.ap[:, j * 128:(j + 1) * 128]
            ktm = KtG.ap[:, j, :]
            vtm = VtG.ap[:, j, :]
            la = la_all.ap[:, n, col:col + 1]
            be = be_all.ap[:, n, col:col + 1]
            nbe = nbe_all.ap[:, n, col:col + 1]
            mU, mS = (UI, SL) if dr == 0 else (LI, SU)
            ps = c["ps"]
            pb = i % 2
            R1, R2, E2, t1, E2m = c["R1"], c["R2"], c["E2"], c["t1"], c["E2m"]
            E1, u, wT, QKm, kd, Egl = c["E1"][pb], c["u"][pb], c["wT"][pb], c["QKm"][pb], c["kd"][pb], c["Egl"][pb]
            ts("dve", R1.ap[:, 0:128], mS, la, ALU.mult, [masks, la_all], [R1])
            cp("pool", R1.ap[:, 128:129], la, [la_all], [R1])
            ts("pool", R2.ap[:, 0:128], mU, la, ALU.mult, [masks, la_all], [R2])
            cp("pool", R2.ap[:, 128:129], la, [la_all], [R2])
            mm(c["pG1"].ap, mU, R1.ap, [masks, R1], [c["pG1"]])
            mm(c["pG2"].ap, mS, R2.ap, [masks, R2], [c["pG2"]])
            act(E1.ap, c["pG1"].ap, AF.Exp, [c["pG1"]], [E1])
            act(E2.ap, c["pG2"].ap, AF.Exp, [c["pG2"]], [E2])
            ts("pool", c["lc"].ap, ci.ap, la, ALU.mult, [ci, la_all], [c["lc"]])
            mm(ps[4].ap[:, 0:2], onesF, c["lc"].ap, [misc, c["lc"]], [ps[4]])
            act(Egl.ap, ps[4].ap[:, 0:2], AF.Exp, [ps[4]], [Egl])
            mm(ps[5].ap, kTt, kTt, [KG], [ps[5]])
            mm(ps[6].ap, kTt, qTt, [KG, QG], [ps[6]])
            N0, N1, M0, M1, PT0, PT1 = c["N0"], c["N1"], c["M0"], c["M1"], c["PT0"], c["PT1"]
            tt("dve", t1.ap[:, 0:128], ps[5].ap, E1.ap[:, 0:128], ALU.mult, [ps[5], E1], [t1])
            stt("dve", N0.ap[:, 0:128], t1.ap[:, 0:128], nbe, mS, ALU.mult, ALU.mult, [t1, nbe_all, masks], [N0])
            tt("pool", E2m.ap[:, 0:128], E2.ap[:, 0:128], mU, ALU.mult, [E2, masks], [E2m])
            tt("dve", QKm.ap, ps[6].ap, E2m.ap[:, 0:128], ALU.mult, [ps[6], E2m], [QKm])
            tr(ps[7].ap, N0.ap[:, 0:128], identF, [N0, misc], [ps[7]])
            cp("act", M0.ap[:, 0:128], ps[7].ap, [ps[7]], [M0])
            tt("dve", PT0.ap[:, 0:128], ps[7].ap, identF, ALU.add, [ps[7], misc], [PT0])
            Ncur, Mcur, Pcur = N0, M0, PT0
            Nnxt, Mnxt, Pnxt = N1, M1, PT1
            for lv in range(1, 6):
                mm(ps[0].ap, Mcur.ap[:, 0:128], Ncur.ap[:, 0:128], [Mcur, Ncur], [ps[0]])
                cp("act", Nnxt.ap[:, 0:128], ps[0].ap, [ps[0]], [Nnxt])
                if lv < 5:
                    mm(ps[1].ap, Ncur.ap[:, 0:128], Mcur.ap[:, 0:128], [Mcur, Ncur], [ps[1]])
                    cp("dve", Mnxt.ap[:, 0:128], ps[1].ap, [ps[1]], [Mnxt])
                mm(ps[2].ap, Nnxt.ap[:, 0:128], Pcur.ap[:, 0:128], [Nnxt, Pcur], [ps[2]])
                if lv < 5:
                    tt("dve", Pnxt.ap[:, 0:128], ps[2].ap, Pcur.ap[:, 0:128], ALU.add, [ps[2], Pcur], [Pnxt])
                else:
                    tt("dve", c["TinvT"][pb].ap, ps[2].ap, Pcur.ap[:, 0:128], ALU.add, [ps[2], Pcur], [c["TinvT"][pb]])
                Ncur, Nnxt = Nnxt, Ncur
                Mcur, Mnxt = Mnxt, Mcur
                Pcur, Pnxt = Pnxt, Pcur
            TinvT = c["TinvT"][pb]
            ts("pool", c["vb"].ap, vtm, be, ALU.mult, [VtG, be_all], [c["vb"]])
            tt("pool", c["bg"].ap, be, E1.ap[:, 128:129], ALU.mult, [be_all, E1], [c["bg"]])
            ts("pool", c["kbg"].ap, ktm, c["bg"].ap, ALU.mult, [KtG, c["bg"]], [c["kbg"]])
            ts("pool", kd.ap, ktm, E2.ap[:, 128:129], ALU.mult, [KtG, E2], [kd])
            mm(ps[3].ap, TinvT.ap, c["vb"].ap, [TinvT, c["vb"]], [ps[3]])
            cp("act", u.ap[:, 0:128], ps[3].ap, [ps[3]], [u])
            mm(ps[4].ap, c["kbg"].ap, TinvT.ap, [TinvT, c["kbg"]], [ps[4]])
            cp("act", wT.ap, ps[4].ap, [ps[4]], [wT])

        def C_scan(c, i):
            hh, dr = c["hh"], c["dr"]
            n = i if dr == 0 else NT - 1 - i
            k = (i // GT) % 2
            j = n % GT
            QG = c["QG"][k]
            qTt = QG.ap[:, j * 128:(j + 1) * 128]
            pb = i % 2
            E1, u, wT, QKm, kd, Egl = c["E1"][pb], c["u"][pb], c["wT"][pb], c["QKm"][pb], c["kd"][pb], c["Egl"][pb]
            ps = c["pss"]
            S_, Sbf, vnew, tmp, o_ = c["S"], c["Sbf"], c["vnew"], c["tmp"], c["o"][pb]
            for cc in ((0, 1) if dr == 0 else (1, 0)):
                pc = slice(cc * 64, cc * 64 + 64)
                mm(ps[0].ap, wT.ap, Sbf.ap, [wT, Sbf], [ps[0]])
                mm(ps[1].ap, qTt, Sbf.ap, [QG, Sbf], [ps[1]])
                tt("dve", vnew.ap[pc, :], u.ap[pc, 0:128], ps[0].ap[pc, :], ALU.subtract, [u, ps[0]], [vnew])
                mm(ps[2].ap, QKm.ap[pc, :], vnew.ap[pc, :], [QKm, vnew], [ps[2]])
                mm(ps[3].ap, kd.ap[pc, :], vnew.ap[pc, :], [kd, vnew], [ps[3]])
                act(tmp.ap[pc, :], ps[1].ap[pc, :], AF.Copy, [ps[1], E1], [tmp], scale=E1.ap[pc, 128:129])
                tt("dve", o_.ap[pc, :], tmp.ap[pc, :], ps[2].ap[pc, :], ALU.add, [tmp, ps[2]], [o_])
                stt("dve", S_.ap, S_.ap, Egl.ap[:, cc:cc + 1], ps[3].ap, ALU.mult, ALU.add, [S_, Egl, ps[3]], [S_])
                cp("act", Sbf.ap, S_.ap, [S_], [Sbf])
            dma("sp", dn_o.ap[chn_idx(c), n * 128:(n + 1) * 128, :], o_.ap, R=[o_], W=[dn_o.b((chn_idx(c), n))])

        def chn_idx(c):
            return c["dr"] * 2 + c["hh"]

        for c in chains:
            C_load(c, 0)
        for i in range(NT + 1):
            if i % GT == 1 and (i // GT + 1) * GT < NT:
                for c in chains:
                    C_load(c, i // GT + 1)
            gens = []
            if i >= 1:
                gens += [lambda c=c, i=i: C_scan(c, i - 1) for c in chains]
            if i < NT:
                gens += [lambda c=c, i=i: C_prep(c, i) for c in chains]
            interleave(gens)

        if "gates_c" in dump:
            gates_c = dscr("gates_c", [128, NT, 12])
            dma("pq", gates_c.ap[:, :, 0:4], la_all.ap, R=[la_all], W=[gates_c.b(0)])
            dma("pq", gates_c.ap[:, :, 4:8], be_all.ap, R=[be_all], W=[gates_c.b(1)])
            dma("pq", gates_c.ap[:, :, 8:12], nbe_all.ap, R=[nbe_all], W=[gates_c.b(2)])
        if stop == 'C':
            raise _Stop()
        SC.phase = 'phD'
        AR.release(BASE)
        dnwt = AR.alloc([128, 128], F32)
        dma("sp", dnwt.ap, dnw.ap, W=[dnwt])
        DG = min(4, NT)
        of_ = [AR.alloc([128, DG, 128], F32) for _ in range(2)]
        ob_ = [AR.alloc([128, DG, 128], F32) for _ in range(2)]
        zg = [AR.alloc([128, DG, 128], F32) for _ in range(2)]
        dtmp = [dict(osum=AR.alloc([128, 128], F32), osq=AR.alloc([128, 128], F32), ms=AR.alloc([128, 1], F32),
                     rs=AR.alloc([128, 1], F32), sz=AR.alloc([128, 128], F32), on=AR.alloc([128, 128], F32),
                     resb=AR.alloc([128, 128], BF16)) for _ in range(DG)]
        mo = [[AR.alloc([128, 128], BF16) for _ in range(DG)] for _ in range(2)]
        p_d = [PS.tile(i, 0, [128, 128], BF16) for i in range(DG)]
        it = 0
        for hh in range(2):
            for g in range(NT // DG):
                k = it % 2
                r0 = g * DG * 128
                dma("sp", of_[k].ap, dn_o.ap[hh].rearrange("(n p) d -> p n d", p=128)[:, g * DG:(g + 1) * DG, :],
                    R=dn_o.all(), W=[of_[k]])
                dma("sp", ob_[k].ap, dn_o.ap[2 + hh].rearrange("(n p) d -> p n d", p=128)[:, g * DG:(g + 1) * DG, :],
                    R=dn_o.all(), W=[ob_[k]])
                dma("sp", zg[k].ap, tm_raw.ap.rearrange("(n p) c -> p n c", p=128)[:, g * DG:(g + 1) * DG, hh * 128:(hh + 1) * 128],
                    R=tm_raw.all(), W=[zg[k]])
                def D_one(j, k=k, hh=hh, g=g, r0=r0):
                    t_ = dtmp[j]
                    osum, osq, ms, rs_, sz, on, resb = (t_[x] for x in ("osum", "osq", "ms", "rs", "sz", "on", "resb"))
                    tt("dve", osum.ap, of_[k].ap[:, j, :], ob_[k].ap[:, j, :], ALU.add, [of_[k], ob_[k]], [osum])
                    op("dve", lambda e: e.memset(ms.ap, 0.0), [], [ms])
                    act(osq.ap, osum.ap, AF.Square, [osum], [osq, ms], accum=ms.ap)
                    act(rs_.ap, ms.ap, AF.Sqrt, [ms, epst], [rs_], bias=epst.ap, scale=1.0 / 128)
                    recip(rs_.ap, rs_.ap, [rs_], [rs_])
                    stt("dve", on.ap, osum.ap, rs_.ap, dnwt.ap, ALU.mult, ALU.mult, [osum, rs_, dnwt], [on])
                    act(sz.ap, zg[k].ap[:, j, :], AF.Silu, [zg[k]], [sz])
                    tt("dve", resb.ap, on.ap, sz.ap, ALU.mult, [on, sz], [resb])
                    pt = p_d[j]
                    tr(pt.ap, resb.ap, identB, [resb, miscb], [pt])
                    m_ = mo[k][j]
                    cp("act", m_.ap, pt.ap, [pt], [m_])
                    c0 = r0 % TQ + j * 128
                    dma("pq", mixT_loc.ap[r0 // TQ, hh * 128:(hh + 1) * 128, c0:c0 + 128], m_.ap, R=[m_],
                        W=[mixT_loc.b(("dn", hh, g, j))])
                interleave([lambda j=j: D_one(j) for j in range(DG)])
                it += 1

        if stop == 'D':
            raise _Stop()
        SC.phase = 'phE'
        AR.release(BASE)
        QT = [AR.alloc([128, S], BF16) for _ in range(2)]
        KT = AR.alloc([128, S], BF16)
        Vt = AR.alloc([128, NT, 128], BF16)
        qw = AR.alloc([128, 1], F32)
        kw_ = AR.alloc([128, 1], F32)
        nrow = AR.alloc([128, 2, 128], F32)
        mx2 = AR.alloc([128, 2], F32)
        nbias = AR.alloc([128, 1], F32)
        dma("sp", qw.ap, qnw.ap, W=[qw])
        dma("sp", kw_.ap, knw.ap, W=[kw_])
        dma("sp", nrow.ap[:, 0, :], qnrow.ap, W=[nrow])
        dma("sp", nrow.ap[:, 1, :], knrow.ap, W=[nrow])
        tt("dve", nrow.ap, nrow.ap, nrow.ap, ALU.mult, [nrow], [nrow])
        op("dve", lambda e: e.tensor_reduce(out=mx2.ap, in_=nrow.ap, axis=AX.X, op=ALU.max), [nrow], [mx2])
        tt("dve", nbias.ap, mx2.ap[:, 0:1], mx2.ap[:, 1:2], ALU.mult, [mx2], [nbias])
        act(nbias.ap, nbias.ap, AF.Sqrt, [nbias], [nbias])
        ts("dve", nbias.ap, nbias.ap, -math.sqrt(128.0), ALU.mult, [nbias], [nbias])
        EM = AR.mark()
        araw = [[AR.alloc([128, 512], F32) for _ in range(3)] for _ in range(2)]
        cst = [[AR.alloc([128, 512], F32) for _ in range(2)] for _ in range(2)]
        asq = AR.alloc([128, 512], F32)
        art = AR.alloc([128, 512], F32)
        axn = AR.alloc([128, 512], F32)
        at1 = AR.alloc([128, 512], F32)
        at2 = AR.alloc([128, 512], F32)
        vst = [AR.alloc([128, 4, 128], F32) for _ in range(2)]
        p_as = PS.tile(0, 0, [128, 512], F32)
        p_ar = PS.tile(1, 0, [128, 512], F32)
        for tb in range(NB):
            k = tb % 2
            for ti in range(3):
                dma("sp", araw[k][ti].ap, cm_raw.ap[6 + ti, :, 2 + tb * 512:2 + (tb + 1) * 512], R=cm_raw.all(),
                    W=[araw[k][ti]])
            dma("sp", cst[k][0].ap, cosT.ap[:, tb * 512:(tb + 1) * 512], W=[cst[k][0]])
            dma("sp", cst[k][1].ap, sinT.ap[:, tb * 512:(tb + 1) * 512], W=[cst[k][1]])
            dma("sp", vst[k].ap, tm_raw.ap.rearrange("(n p) c -> p n c", p=128)[:, tb * 4:(tb + 1) * 4, 256:384],
                R=tm_raw.all(), W=[vst[k]])
            cp("pool", Vt.ap[:, tb * 4:(tb + 1) * 4, :], vst[k].ap, [vst[k]], [Vt])
            for ti in range(3):
                x_ = araw[k][ti]
                w_ = qw if ti < 2 else kw_
                dst = QT[ti] if ti < 2 else KT
                tt("pool", asq.ap, x_.ap, x_.ap, ALU.mult, [x_], [asq])
                mm(p_as.ap, onesF, asq.ap, [misc, asq], [p_as])
                act(art.ap, p_as.ap, AF.Sqrt, [p_as, epst], [art], bias=epst.ap, scale=1.0 / 128)
                recip(art.ap, art.ap, [art], [art])
                stt("dve", axn.ap, x_.ap, w_.ap, art.ap, ALU.mult, ALU.mult, [x_, w_, art], [axn])
                mm(p_ar.ap, rotF, axn.ap, [misc, axn], [p_ar])
                tt("pool", at1.ap, axn.ap, cst[k][0].ap, ALU.mult, [axn, cst[k][0]], [at1])
                tt("dve", at2.ap, p_ar.ap, cst[k][1].ap, ALU.mult, [p_ar, cst[k][1]], [at2])
                tt("dve", dst.ap[:, tb * 512:(tb + 1) * 512], at1.ap, at2.ap, ALU.add, [at1, at2], [dst])
        AR.release(EM)
        NPT = 4
        ptile = [AR.alloc([128, 512], BF16) for _ in range(NPT)]
        racc = [AR.alloc([128, 512], F32) for _ in range(2)]
        rsum = AR.alloc([128, 512], F32)
        rinv = AR.alloc([128, 512], F32)
        otn = [AR.alloc([128, 512], BF16) for _ in range(2)]
        p_st = [PS.tile(i, 0, [128, 512], F32) for i in range(4)]
        p_ot = [PS.tile(4 + i, 0, [128, 512], F32) for i in range(2)]
        p_rs = PS.tile(6, 0, [128, 512], F32)
        sc_att = 128.0 ** -0.5
        qi = 0
        for h in range(2):
            for qt in range(NB):
                pot = p_ot[qi % 2]
                qs = QT[h].ap[:, qt * 512:(qt + 1) * 512]
                for kt in range(NT):
                    pst = p_st[kt % 4]
                    mm(pst.ap, KT.ap[:, kt * 128:(kt + 1) * 128], qs, [KT, QT[h]], [pst])
                    pt_ = ptile[kt % NPT]
                    act(pt_.ap, pst.ap, AF.Exp, [pst, nbias], [pt_], bias=nbias.ap, scale=sc_att)
                    mm(pot.ap, Vt.ap[:, kt, :], pt_.ap, [Vt, pt_], [pot], start=(kt == 0), stop=(kt == NT - 1))
                    ra = racc[kt % 2]
                    eng = "dve" if kt % 2 == 0 else "pool"
                    if kt < 2:
                        cp(eng, ra.ap, pt_.ap, [pt_], [ra])
                    else:
                        tt(eng, ra.ap, ra.ap, pt_.ap, ALU.add, [ra, pt_], [ra])
                if NT > 1:
                    tt("dve", rsum.ap, racc[0].ap, racc[1].ap, ALU.add, [racc[0], racc[1]], [rsum])
                else:
                    cp("dve", rsum.ap, racc[0].ap, [racc[0]], [rsum])
                mm(p_rs.ap, onesF, rsum.ap, [misc, rsum], [p_rs])
                recip(rinv.ap, p_rs.ap, [p_rs], [rinv])
                ot = otn[qi % 2]
                tt("dve", ot.ap, pot.ap, rinv.ap, ALU.mult, [pot, rinv], [ot])
                dma("sp", mixT_loc.ap[(qt * 512) // TQ, 256 + h * 128:256 + (h + 1) * 128,
                                      (qt * 512) % TQ:(qt * 512) % TQ + 512], ot.ap, R=[ot],
                    W=[mixT_loc.b(("att", h, qt))])
                qi += 1

        if stop == 'E':
            raise _Stop()
        SC.phase = 'phF'
        for q in range(NQ):
            SC.cc(lambda e, q=q: e.collective_compute("AllGather", ALU.bypass, replica_groups=GRP,
                                                      ins=[mixT_loc.ap[q]], outs=[mixT_full.ap[q]]),
                  R=mixT_loc.all(), W=[mixT_full.b()])

        if stop == 'F':
            raise _Stop()
        SC.phase = 'phG'
        AR.release(BASE)
        WO = [AR.alloc([128, D], BF16) for _ in range(16)]
        wst = [AR.alloc([128, D], F32) for _ in range(2)]
        for cc in range(16):
            st = wst[cc % 2]
            dma("sp", st.ap, w_out.ap[cc * 128:(cc + 1) * 128, :], W=[st])
            cp("pool", WO[cc].ap, st.ap, [st], [WO[cc]])
        ln2t = AR.alloc([128, D], F32)
        wrt = AR.alloc([128, 16, NE], F32)
        dma("sp", ln2t.ap, ln2bc.ap, W=[ln2t])
        dma("sp", wrt.ap, wr.ap, W=[wrt])
        MB = min(512, TPC)
        mixb = [AR.alloc([128, 16, MB], BF16) for _ in range(2)]
        xt_ = [AR.alloc([128, D], F32) for _ in range(2)]
        x1t = [AR.alloc([128, D], F32) for _ in range(2)]
        h2f = AR.alloc([128, D], F32)
        h2b = [AR.alloc([128, D], BF16) for _ in range(2)]
        h2T = AR.alloc([128, 16, 128], F32)
        gsq = AR.alloc([128, D], BF16)
        gms = AR.alloc([128, 1], F32)
        grs = AR.alloc([128, 1], F32)
        lmx = AR.alloc([128, 1], F32)
        lsum = AR.alloc([128, 1], F32)
        lex = AR.alloc([128, NE], F32)
        afft = [AR.alloc([128, NE], F32) for _ in range(2)]
        p_y = [PS.tile(i, 0, [128, 512], F32) for i in range(4)]
        p_t = [PS.tile(4 + i, 0, [128, 4, 128], F32) for i in range(2)]
        p_l = PS.tile(6, 0, [128, NE], F32)
        mfv = mixT_full.ap.rearrange("q (cc p) t -> q cc p t", p=128)

        def G_loadmix(mbi):
            def f(e, mbi=mbi):
                qd = dynval(e, TPC // TQ, (mbi * MB) // TQ)
                o_ = (mbi * MB) % TQ
                return e.dma_start(out=mixb[mbi % 2].ap,
                                   in_=mfv[bass.ds(qd, 1), :, :, o_:o_ + MB].rearrange("q cc p t -> p (q cc) t"))
            op("sp", f, R=[mixT_full.b()], W=[mixb[mbi % 2]], dma=True)

        G_loadmix(0)
        for i in range(NTC):
            mbi, mo_ = (i * 128) // MB, (i * 128) % MB
            if mo_ == 0 and (mbi + 1) * MB < TPC:
                G_loadmix(mbi + 1)
            k = i % 2
            dma("sp", xt_[k].ap, xtok.ap[i * 128:(i + 1) * 128, :], W=[xt_[k]])
            mb_ = mixb[mbi % 2]
            for db in range(4):
                for cc in range(16):
                    mm(p_y[db].ap, mb_.ap[:, cc, mo_:mo_ + 128], WO[cc].ap[:, db * 512:(db + 1) * 512], [mb_, WO[cc]],
                       [p_y[db]], start=(cc == 0), stop=(cc == 15))
            for db in range(4):
                tt("dve", x1t[k].ap[:, db * 512:(db + 1) * 512], p_y[db].ap, xt_[k].ap[:, db * 512:(db + 1) * 512],
                   ALU.add, [p_y[db], xt_[k]], [x1t[k]])
            dma("pq", x1_loc.ap[i * 128:(i + 1) * 128, :], x1t[k].ap, R=[x1t[k]], W=[x1_loc.b(i)])
            op("dve", lambda e: e.memset(gms.ap, 0.0), [], [gms])
            act(gsq.ap, x1t[k].ap, AF.Square, [x1t[k]], [gsq, gms], accum=gms.ap)
            act(grs.ap, gms.ap, AF.Sqrt, [gms, epst], [grs], bias=epst.ap, scale=1.0 / D)
            recip(grs.ap, grs.ap, [grs], [grs])
            stt("dve", h2f.ap, x1t[k].ap, grs.ap, ln2t.ap, ALU.mult, ALU.mult, [x1t[k], grs, ln2t], [h2f])
            cp("pool", h2b[k].ap, h2f.ap, [h2f], [h2b[k]])
            dma("pq", h2_loc.ap[i * 128:(i + 1) * 128, :], h2b[k].ap, R=[h2b[k]], W=[h2_loc.b(i)])
            for q4 in range(4):
                pt = p_t[q4 % 2]
                for j in range(4):
                    dc = q4 * 4 + j
                    tr(pt.ap[:, j, :], h2f.ap[:, dc * 128:(dc + 1) * 128], identF, [h2f, misc], [pt])
                cp("act" if q4 % 2 == 0 else "dve", h2T.ap[:, q4 * 4:(q4 + 1) * 4, :], pt.ap, [pt], [h2T])
            for dc in range(16):
                mm(p_l.ap, h2T.ap[:, dc, :], wrt.ap[:, dc, :], [h2T, wrt], [p_l], start=(dc == 0), stop=(dc == 15))
            op("dve", lambda e: e.tensor_reduce(out=lmx.ap, in_=p_l.ap, axis=AX.X, op=ALU.max), [p_l], [lmx])
            ts("dve", lmx.ap, lmx.ap, -1.0, ALU.mult, [lmx], [lmx])
            op("dve", lambda e: e.memset(lsum.ap, 0.0), [], [lsum])
            act(lex.ap, p_l.ap, AF.Exp, [p_l, lmx], [lex, lsum], bias=lmx.ap, accum=lsum.ap)
            recip(lsum.ap, lsum.ap, [lsum], [lsum])
            ts("dve", afft[k].ap, lex.ap, lsum.ap, ALU.mult, [lex, lsum], [afft[k]])
            dma("pq", aff_loc.ap[i * 128:(i + 1) * 128, :], afft[k].ap, R=[afft[k]], W=[aff_loc.b(i)])

        if stop == 'G':
            raise _Stop()
        SC.phase = 'phH'
        SC.cc(lambda e: e.collective_compute("AllGather", ALU.bypass, replica_groups=GRP,
                                             ins=[aff_loc.ap], outs=[aff_full.ap]),
              R=aff_loc.all(), W=[aff_full.b()])
        for q in range(TPC // HQ):
            SC.cc(lambda e, q=q: e.collective_compute("AllGather", ALU.bypass, replica_groups=GRP,
                                                      ins=[h2_loc.ap[q * HQ:(q + 1) * HQ, :]],
                                                      outs=[h2_full.ap[q * 4 * HQ:(q + 1) * 4 * HQ, :]]),
                  R=h2_loc.all(), W=[h2_full.b()])

        if stop == 'H':
            raise _Stop()
        SC.phase = 'phI'
        AR.release(BASE)
        A3 = AR.alloc([128, NT, NE], F32)
        AE = AR.alloc([128, NE, NT], F32)
        cmpt = AR.alloc([128, NE, NT], F32)
        selb = AR.alloc([128, NE, NT], BF16)
        cum = AR.alloc([128, NE, NT], F32)
        tta = AR.alloc([128, NE, NT], F32)
        ttb = AR.alloc([128, NE, NT], F32)
        pos = AR.alloc([128, NE, NT], F32)
        posi = AR.alloc([128, NE, NT], I32)
        posgi = AR.alloc([128, NE, NT], I32)
        gsf = AR.alloc([128, NE, NT], F32)
        lo = AR.alloc([128, NE, 1], F32)
        hi = AR.alloc([128, NE, 1], F32)
        mid = AR.alloc([128, NE, 1], F32)
        dlt = AR.alloc([128, NE, 1], F32)
        ge = AR.alloc([128, NE, 1], F32)
        cntp = AR.alloc([128, NE], F32)
        ecap = AR.alloc([128, NE, 1], F32)
        p_c = PS.tile(0, 0, [128, NE], F32)
        dma("sp", ecap.ap[:, :, 0], cecap.ap, W=[ecap])
        for n0 in range(0, NT, 16):
            dma("sp", A3.ap[:, n0:n0 + 16, :], aff_full.ap.rearrange("(n p) e -> p n e", p=128)[:, n0:n0 + 16, :],
                R=[aff_full.b()], W=[A3])
        cp("dve", AE.ap, A3.ap.rearrange("p n e -> p e n"), [A3], [AE])
        op("dve", lambda e: e.memset(lo.ap, 0.0), [], [lo])
        op("dve", lambda e: e.memset(hi.ap, 1.0), [], [hi])
        NESH = [128, NE, NT]
        for itr in range(34):
            tt("dve", mid.ap, lo.ap, hi.ap, ALU.add, [lo, hi], [mid])
            ts("dve", mid.ap, mid.ap, 0.5, ALU.mult, [mid], [mid])
            tt("dve", cmpt.ap, AE.ap, mid.ap.to_broadcast(NESH), ALU.is_gt, [AE, mid], [cmpt])
            op("dve", lambda e: e.tensor_reduce(out=cntp.ap, in_=cmpt.ap, axis=AX.X, op=ALU.add), [cmpt], [cntp])
            mm(p_c.ap, onesF, cntp.ap, [misc, cntp], [p_c])
            ts("dve", ge.ap[:, :, 0], p_c.ap, float(CAP) - 0.5, ALU.is_gt, [p_c], [ge])
            tt("dve", dlt.ap, mid.ap, lo.ap, ALU.subtract, [mid, lo], [dlt])
            tt("dve", dlt.ap, dlt.ap, ge.ap, ALU.mult, [dlt, ge], [dlt])
            tt("dve", lo.ap, lo.ap, dlt.ap, ALU.add, [lo, dlt], [lo])
            tt("dve", dlt.ap, hi.ap, mid.ap, ALU.subtract, [hi, mid], [dlt])
            tt("dve", dlt.ap, dlt.ap, ge.ap, ALU.mult, [dlt, ge], [dlt])
            tt("dve", hi.ap, mid.ap, dlt.ap, ALU.add, [mid, dlt], [hi])
        tt("dve", cmpt.ap, AE.ap, lo.ap.to_broadcast(NESH), ALU.is_gt, [AE, lo], [cmpt])
        cp("pool", selb.ap, cmpt.ap, [cmpt], [selb])
        tt("pool", gsf.ap, AE.ap, cmpt.ap, ALU.mult, [AE, cmpt], [gsf])
        dma("pq", gs_d.ap, gsf.ap, R=[gsf], W=[gs_d.b()])
        selv = selb.ap.rearrange("p e n -> p (e n)")
        cumv = cum.ap.rearrange("p e n -> p (e n)")
        ttav = tta.ap.rearrange("p e n -> p (e n)")
        NEN = NE * NT
        CW = min(512, NEN)
        pcs = [PS.tile(1 + i, 0, [128, CW], F32) for i in range(2)]
        for j in range(NEN // CW):
            pt = pcs[j % 2]
            mm(pt.ap, UFb, selv[:, j * CW:(j + 1) * CW], [miscb, selb], [pt])
            cp("act", cumv[:, j * CW:(j + 1) * CW], pt.ap, [pt], [cum])
            pt2 = pcs[(j + 1) % 2]
            mm(pt2.ap, onesB, selv[:, j * CW:(j + 1) * CW], [miscb, selb], [pt2])
            cp("dve", ttav[:, j * CW:(j + 1) * CW], pt2.ap, [pt2], [tta])
        cp("pool", pos.ap, tta.ap, [tta], [pos])
        src, dst = tta, ttb
        sh = 1
        while sh < NT:
            cp("dve", dst.ap[:, :, 0:sh], src.ap[:, :, 0:sh], [src], [dst])
            tt("dve", dst.ap[:, :, sh:NT], src.ap[:, :, sh:NT], src.ap[:, :, 0:NT - sh], ALU.add, [src], [dst])
            src, dst = dst, src
            sh *= 2
        tt("dve", dst.ap, src.ap, pos.ap, ALU.subtract, [src, pos], [dst])
        tt("dve", pos.ap, cum.ap, dst.ap, ALU.add, [cum, dst], [pos])
        ts("dve", pos.ap, pos.ap, -1.0 - BIG, ALU.add, [pos], [pos])
        tt("dve", pos.ap, pos.ap, cmpt.ap, ALU.mult, [pos, cmpt], [pos])
        ts("dve", pos.ap, pos.ap, BIG, ALU.add, [pos], [pos])
        cp("dve", posi.ap, pos.ap, [pos], [posi])
        op("dve", lambda e: e.memset(tta.ap, 0.0), [], [tta])
        for kq in range(1, CAP // HQ):
            ts("dve", ttb.ap, pos.ap, kq * HQ - 0.5, ALU.is_gt, [pos], [ttb])
            tt("dve", tta.ap, tta.ap, ttb.ap, ALU.add, [tta, ttb], [tta])
        stt("dve", pos.ap, tta.ap, 3.0 * HQ, pos.ap, ALU.mult, ALU.add, [tta, pos], [pos])
        tt("dve", pos.ap, pos.ap, ecap.ap.to_broadcast(NESH), ALU.add, [pos, ecap], [pos])
        cp("dve", posgi.ap, pos.ap, [pos], [posgi])
        dma("pq", pos_d.ap, posi.ap, R=[posi], W=[pos_d.b()])
        dma("pq", posg_d.ap, posgi.ap, R=[posgi], W=[posg_d.b()])

        if stop == 'I':
            raise _Stop()
        SC.phase = 'phJ'
        AR.release(BASE)
        mypos = AR.alloc([128, 4, NT], I32)

        def J_ld(e):
            return e.dma_start(out=mypos.ap, in_=pos_d.ap[:, bass.ds(dynval(e, 4, 0), 4), :])
        op("pq", J_ld, R=[pos_d.b()], W=[mypos], dma=True)
        hrow = [AR.alloc([128, D], BF16) for _ in range(3)]
        for n in range(NT):
            hr = hrow[n % 3]
            rr_, tl0 = n // NTC, (n % NTC) * 128
            row0 = (tl0 // HQ) * 4 * HQ + rr_ * HQ + tl0 % HQ
            dma("sp", hr.ap, h2_full.ap[row0:row0 + 128, :], R=[h2_full.b()], W=[hr])
            for j in range(4):
                def f(e, j=j, n=n, hr=hr):
                    return e.indirect_dma_start(out=xs[j].ap, out_offset=bass.IndirectOffsetOnAxis(ap=mypos.ap[:, j, n:n + 1], axis=0),
                                                in_=hr.ap, in_offset=None, bounds_check=bcreg(e, CAP - 1), oob_is_err=False)
                op("pq", f, R=[hr, mypos], W=[xs[j].b(n)], dma=True)

        if stop == 'J':
            raise _Stop()
        SC.phase = 'phK'
        AR.release(BASE)
        xsT = AR.alloc([128, 16, CAP], BF16)
        hmT = AR.alloc([128, 16, CAP], BF16)
        KM = AR.mark()
        p_x = [PS.tile(i, 0, [128, 4, 128], BF16) for i in range(2)]
        p_a = [PS.tile(2 + i, 0, [128, TBK], F32) for i in range(2)]
        p_u = [PS.tile(4 + i, 0, [128, TBK], F32) for i in range(2)]
        p_yk = [PS.tile(6 + i, 0, [128, 256], F32) for i in range(2)]
        for j in range(4):
            AR.release(KM)
            xrow = [AR.alloc([128, D], BF16) for _ in range(2)]
            for t_ in range(CAP // 128):
                xr = xrow[t_ % 2]
                dma("sp", xr.ap, xs[j].ap[t_ * 128:(t_ + 1) * 128, :], R=xs[j].all(), W=[xr])
                for q4 in range(4):
                    pt = p_x[q4 % 2]
                    for jj in range(4):
                        dc = q4 * 4 + jj
                        tr(pt.ap[:, jj, :], xr.ap[:, dc * 128:(dc + 1) * 128], identB, [xr, miscb], [pt])
                    cp("act" if q4 % 2 == 0 else "dve", xsT.ap[:, q4 * 4:(q4 + 1) * 4, t_ * 128:(t_ + 1) * 128], pt.ap,
                       [pt], [xsT])
            AR.release(KM)
            gst = [AR.alloc([128, 16, 128], F32) for _ in range(2)]
            ust = [AR.alloc([128, 16, 128], F32) for _ in range(2)]
            gbf = [AR.alloc([128, 16, 128], BF16) for _ in range(2)]
            ubf = [AR.alloc([128, 16, 128], BF16) for _ in range(2)]
            sa = [AR.alloc([128, TBK], F32) for _ in range(2)]
            wgv = wg.ap[j].rearrange("(dc p) f -> p dc f", p=128)
            wuv = wu.ap[j].rearrange("(dc p) f -> p dc f", p=128)

            def K_ldw(fc):
                k = fc % 2
                dma("sp", gst[k].ap, wgv[:, :, fc * 128:(fc + 1) * 128], W=[gst[k]])
                dma("sp", ust[k].ap, wuv[:, :, fc * 128:(fc + 1) * 128], W=[ust[k]])
                cp("pool", gbf[k].ap, gst[k].ap, [gst[k]], [gbf[k]])
                cp("pool", ubf[k].ap, ust[k].ap, [ust[k]], [ubf[k]])
            K_ldw(0)
            ii = 0
            for fc in range(16):
                if fc + 1 < 16:
                    K_ldw(fc + 1)
                k = fc % 2
                for tb in range(CAP // TBK):
                    pa, pu = p_a[ii % 2], p_u[ii % 2]
                    for dc in range(16):
                        mm(pa.ap, gbf[k].ap[:, dc, :], xsT.ap[:, dc, tb * TBK:(tb + 1) * TBK], [gbf[k], xsT], [pa],
                           start=(dc == 0), stop=(dc == 15))
                    for dc in range(16):
                        mm(pu.ap, ubf[k].ap[:, dc, :], xsT.ap[:, dc, tb * TBK:(tb + 1) * TBK], [ubf[k], xsT], [pu],
                           start=(dc == 0), stop=(dc == 15))
                    s_ = sa[ii % 2]
                    act(s_.ap, pa.ap, AF.Silu, [pa], [s_])
                    tt("dve", hmT.ap[:, fc, tb * TBK:(tb + 1) * TBK], s_.ap, pu.ap, ALU.mult, [s_, pu], [hmT])
                    ii += 1
            AR.release(KM)
            dst_ = [AR.alloc([128, 16, 256], F32) for _ in range(2)]
            dbf = [AR.alloc([128, 16, 256], BF16) for _ in range(2)]
            yst = [AR.alloc([128, 256], BF16) for _ in range(3)]
            wdv = wd.ap[j].rearrange("(fc p) d -> p fc d", p=128)

            def K_ldd(db):
                k = db % 2
                dma("sp", dst_[k].ap, wdv[:, :, db * 256:(db + 1) * 256], W=[dst_[k]])
                cp("pool", dbf[k].ap, dst_[k].ap, [dst_[k]], [dbf[k]])
            K_ldd(0)
            ii = 0
            for db in range(8):
                if db + 1 < 8:
                    K_ldd(db + 1)
                k = db % 2
                for t_ in range(CAP // 128):
                    py = p_yk[ii % 2]
                    for fc in range(16):
                        mm(py.ap, hmT.ap[:, fc, t_ * 128:(t_ + 1) * 128], dbf[k].ap[:, fc, :], [hmT, dbf[k]], [py],
                           start=(fc == 0), stop=(fc == 15))
                    ys = yst[ii % 3]
                    cp("act" if ii % 2 == 0 else "dve", ys.ap, py.ap, [py], [ys])
                    dma("pq", y_loc.ap[j * CAP + t_ * 128:j * CAP + (t_ + 1) * 128, db * 256:(db + 1) * 256], ys.ap,
                        R=[ys], W=[y_loc.b((j, db, t_))])
                    ii += 1

        if stop == 'K':
            raise _Stop()
        SC.phase = 'phL'
        for q in range(4 * CAP // HQ):
            SC.cc(lambda e, q=q: e.collective_compute("AllGather", ALU.bypass, replica_groups=GRP,
                                                      ins=[y_loc.ap[q * HQ:(q + 1) * HQ, :]],
                                                      outs=[y_full.ap[q * 4 * HQ:(q + 1) * 4 * HQ, :]]),
                  R=y_loc.all(), W=[y_full.b()])

        if stop == 'L':
            raise _Stop()
        SC.phase = 'phM'
        AR.release(BASE)
        cpos = AR.alloc([128, NE, NTC], I32)
        cgs = AR.alloc([128, NE, NTC], F32)

        def M_ld1(e):
            return e.dma_start(out=cpos.ap, in_=posg_d.ap[:, :, bass.ds(dynval(e, NTC, 0), NTC)])

        def M_ld2(e):
            return e.dma_start(out=cgs.ap, in_=gs_d.ap[:, :, bass.ds(dynval(e, NTC, 0), NTC)])
        op("pq", M_ld1, R=[posg_d.b()], W=[cpos], dma=True)
        op("pq", M_ld2, R=[gs_d.b()], W=[cgs], dma=True)
        NGB = 32
        gbuf = [AR.alloc([128, D], BF16) for _ in range(NGB)]
        for gb in gbuf:
            op("pool", lambda e, gb=gb: e.memset(gb.ap, 0.0), [], [gb])
        accm = [AR.alloc([128, D], F32) for _ in range(2)]
        gi = 0
        for i in range(NTC):
            a0 = accm[i % 2]
            dma("sp", a0.ap, x1_loc.ap[i * 128:(i + 1) * 128, :], R=[x1_loc.b(i)], W=[a0])
            for e_ in range(NE):
                gb = gbuf[gi % NGB]
                gi += 1

                def f(e, gb=gb, e_=e_, i=i):
                    return e.indirect_dma_start(out=gb.ap, out_offset=None, in_=y_full.ap,
                                                in_offset=bass.IndirectOffsetOnAxis(ap=cpos.ap[:, e_, i:i + 1], axis=0),
                                                bounds_check=bcreg(e, NE * CAP - 1), oob_is_err=False)
                op("pq", f, R=[y_full.b(), cpos], W=[gb], dma=True)
                stt("dve", a0.ap, gb.ap, cgs.ap[:, e_, i:i + 1], a0.ap, ALU.mult, ALU.add, [gb, cgs, a0], [a0])
            dma("sp", out.ap[i * 128:(i + 1) * 128, :], a0.ap, R=[a0], W=[out.b(i)])

    except _Stop:
        pass
    for nm in dump:
        src = scr[nm]
        dbg = nc.dram_tensor('dbg_' + nm, list(src.ap.shape), src.ap.dtype, kind='ExternalOutput').ap()
        dma('sp', dbg, src.ap, R=src.all(), W=[DT(None).b()])
    SC.emit()
    return nc, SC


def _consts(S):
    CAP = 2 * S // NE
    p = np.arange(128)[:, None]
    f = np.arange(128)[None, :]
    same = (p // 64) == (f // 64)
    cmask = np.stack([(p >= f) & same, (p > f) & same, (p <= f) & same, (p < f) & same], 1).astype(np.float32)
    ident = np.eye(128, dtype=np.float32)
    rot = np.zeros((128, 128), np.float32)
    for m in range(128):
        blk = m // 32
        if blk % 2 == 0:
            rot[m + 32, m] = -1.0
        else:
            rot[m - 32, m] = 1.0
    uf = (p <= f).astype(np.float32)
    ones = np.ones((128, 128), np.float32)
    cmisc = np.stack([ident, rot, uf, ones], 1).astype(np.float32)
    cci = np.stack([(np.arange(128) < 64), (np.arange(128) >= 64)], 1).astype(np.float32)
    cecap = np.tile(np.array([(e % 4) * 4 * CAP + (e // 4) * 256 for e in range(NE)], np.float32)[None, :], (128, 1))
    t = np.arange(S)
    row = (t // 64).astype(np.float32)
    col = (t % 64).astype(np.float32)
    nf = 32
    inv = (10000.0 ** (-np.arange(nf, dtype=np.float32) / nf)).astype(np.float32)
    ang_r = row[:, None] * inv
    ang_c = col[:, None] * inv
    ang = np.concatenate([ang_r, ang_r, ang_c, ang_c], -1).astype(np.float32)
    cosT = np.ascontiguousarray(np.cos(ang).T.astype(np.float32))
    sinT = np.ascontiguousarray(np.sin(ang).T.astype(np.float32))
    return dict(cmask=np.ascontiguousarray(cmask), cmisc=np.ascontiguousarray(cmisc), cci=cci, cecap=cecap,
                cosT=cosT, sinT=sinT)


_CACHE = {}


def kernel(x, ln1_w, w_in, conv_w, a_log, dt_bias, dn_norm_w, q_norm_w, k_norm_w,
           w_out, ln2_w, w_router, w_gate, w_up, w_down):
    x = np.asarray(x, np.float32)
    B, S, _ = x.shape
    assert B == 2
    TPC = S // 4
    f32 = lambda a: np.ascontiguousarray(np.asarray(a, np.float32))
    ln1_w, w_in, conv_w, a_log, dt_bias = f32(ln1_w)[0], np.asarray(w_in, np.float32)[0], f32(conv_w)[0], f32(a_log)[0], f32(dt_bias)[0]
    dn_norm_w, q_norm_w, k_norm_w = f32(dn_norm_w)[0], f32(q_norm_w)[0], f32(k_norm_w)[0]
    w_out, ln2_w, w_router = np.asarray(w_out, np.float32)[0], f32(ln2_w)[0], f32(w_router)[0]
    w_gate, w_up, w_down = np.asarray(w_gate, np.float32)[0], np.asarray(w_up, np.float32)[0], np.asarray(w_down, np.float32)[0]
    if S not in _CACHE:
        _CACHE[S] = build(S)
    nc, _ = _CACHE[S]
    cst = _consts(S)
    xTs = [np.ascontiguousarray(x[b].T) for b in range(B)]
    perm = []
    for r in range(4):
        for blk in (2 * r, 2 * r + 1):
            perm.extend(range(blk * 128, (blk + 1) * 128))
        for blk in (2 * r, 2 * r + 1):
            perm.extend(range(1024 + blk * 128, 1024 + (blk + 1) * 128))
    w_out_p = np.ascontiguousarray(w_out[np.array(perm)])
    ln1_l = np.ascontiguousarray(ln1_w.reshape(16, 128).T)
    ln2bc = np.ascontiguousarray(np.tile(ln2_w[None, :], (128, 1)))
    wr_l = np.ascontiguousarray(w_router.reshape(16, 128, NE).transpose(1, 0, 2))
    dnw = np.ascontiguousarray(np.tile(dn_norm_w[None, :], (128, 1)))
    qnrow = np.ascontiguousarray(np.tile(q_norm_w[None, :], (128, 1)))
    knrow = np.ascontiguousarray(np.tile(k_norm_w[None, :], (128, 1)))
    in_maps = []
    for c in range(8):
        b, r = c // 4, c % 4
        kv = r // 2
        h0, h1 = 2 * r, 2 * r + 1
        sl = lambda off, h: list(range(off + h * 128, off + (h + 1) * 128))
        cols = (sl(0, h0) + sl(0, h1) + sl(1024, h0) + sl(1024, h1) + sl(2048, h0) + sl(2048, h1)
                + sl(4128, h0) + sl(4128, h1) + sl(5152, kv)
                + sl(3072, h0) + sl(3072, h1) + sl(5408, kv)
                + [4096 + h0, 4096 + h1, 4104 + h0, 4104 + h1, 4112 + h0, 4112 + h1, 4120 + h0, 4120 + h1])
        cols = np.array(cols)
        cch = np.array(sl(0, h0) + sl(0, h1) + sl(1024, h0) + sl(1024, h1) + sl(2048, h0) + sl(2048, h1))
        convw = np.ascontiguousarray(conv_w[:, cch].reshape(5, 6, 128).transpose(2, 1, 0))
        sel4 = lambda a: np.ascontiguousarray(np.tile(np.array([a[0, h0], a[0, h1], a[1, h0], a[1, h1]], np.float32)[None, :], (128, 1)))
        m = dict(
            xT=xTs[b], xtok=np.ascontiguousarray(x[b, r * TPC:(r + 1) * TPC]),
            w_in=np.ascontiguousarray(w_in[:, cols]), ln1=ln1_l, convw=convw,
            alog=sel4(a_log), dtb=sel4(dt_bias), dnw=dnw,
            qnw=np.ascontiguousarray(q_norm_w[:, None]), knw=np.ascontiguousarray(k_norm_w[:, None]),
            qnrow=qnrow, knrow=knrow, w_out=w_out_p, ln2bc=ln2bc, wr=wr_l,
            wg=np.ascontiguousarray(w_gate[4 * r:4 * r + 4]), wu=np.ascontiguousarray(w_up[4 * r:4 * r + 4]),
            wd=np.ascontiguousarray(w_down[4 * r:4 * r + 4]),
        )
        m.update(cst)
        in_maps.append(m)
    res = run_bass_kernel_spmd(nc, in_maps, core_ids=list(range(8)))
    y = np.empty((B, S, D), np.float32)
    for c in range(8):
        b, r = c // 4, c % 4
        y[b, r * TPC:(r + 1) * TPC] = res.results[c]["out"]
    return y
```
